# Optimizing a Trainium2 kernel written in Bass

```python
import jax, jax.numpy as jnp
from jax import lax
import numpy as np

D_MODEL = 2048
BATCH = 4
SEQ = 4096
DEPTH = 1

NORM_EPS = 1e-6
MIX_WIDTH = D_MODEL
GLA_HEADS = 4
GLA_WIDTH = MIX_WIDTH // 2
GLA_DV = GLA_WIDTH // GLA_HEADS
GLA_DK = GLA_DV // 2
GLA_GATE_RANK = 16
GLA_TAU = 16.0
GLA_CHUNK = 64
DIL_HD = 128
DIL_WIDTH = MIX_WIDTH - GLA_WIDTH
DIL_HEADS = DIL_WIDTH // DIL_HD
DIL_PATTERNS = ((128, 1), (512, 4), (2048, 16))
DIL_BLOCK = 64
ROPE_THETA = 10000.0
NEG_INF = -1e30
PEER_HEADS = 8
PEER_NKEYS = 128
PEER_NEXPERTS = PEER_NKEYS * PEER_NKEYS
PEER_QDIM = 256
PEER_TOPK = 16
PEER_TOKEN_BLOCK = 128
IN_SIZES = (GLA_HEADS * GLA_DK, GLA_HEADS * GLA_DK, GLA_WIDTH, GLA_WIDTH,
            GLA_GATE_RANK, GLA_GATE_RANK, DIL_WIDTH, DIL_WIDTH, DIL_WIDTH)
IN_WIDTH = sum(IN_SIZES)

kernel_name = "hybrid_gla_dilated_peer_encoder_block"


def rms_norm(x, g):
    xf = x.astype(jnp.float32)
    y = xf * lax.rsqrt(jnp.mean(xf * xf, axis=-1, keepdims=True) + NORM_EPS)
    return (y * g.astype(jnp.float32)).astype(x.dtype)


def split_cols(t, sizes):
    outs, start = [], 0
    for s in sizes:
        outs.append(t[..., start:start + s])
        start += s
    return outs


def apply_rope(t, positions):
    hd = t.shape[-1]
    half = hd // 2
    inv_freq = jnp.power(ROPE_THETA, -jnp.arange(half, dtype=jnp.float32) * 2.0 / hd)
    ang = positions.astype(jnp.float32)[..., None] * inv_freq
    cos = jnp.cos(ang)[:, :, None, :]
    sin = jnp.sin(ang)[:, :, None, :]
    tf = t.astype(jnp.float32)
    t1, t2 = tf[..., :half], tf[..., half:]
    return jnp.concatenate([t1 * cos - t2 * sin, t1 * sin + t2 * cos], axis=-1).astype(t.dtype)


def gla_chunked(q, k, v, log_a):
    B, S, H, dk = q.shape
    dv = v.shape[-1]
    C = GLA_CHUNK
    n = S // C

    def to_chunks(t):
        return t.reshape(B, n, C, H, t.shape[-1]).transpose(1, 0, 3, 2, 4)

    qc, kc, vc = to_chunks(q), to_chunks(k), to_chunks(v.astype(jnp.float32))
    b = jnp.cumsum(to_chunks(log_a).astype(jnp.float32), axis=-2)
    b_last = b[..., -1:, :]
    q_dec = qc.astype(jnp.float32) * jnp.exp(b)
    k_inv = kc.astype(jnp.float32) * jnp.exp(-b)
    k_to_end = kc.astype(jnp.float32) * jnp.exp(b_last - b)
    lower = jnp.tril(jnp.ones((C, C), dtype=bool))
    attn = jnp.where(lower, jnp.einsum('nbhid,nbhjd->nbhij', q_dec, k_inv), 0.0)
    o_intra = jnp.einsum('nbhij,nbhjv->nbhiv', attn, vc)

    def step(state, inp):
        kt, vt, decay = inp
        new = state * decay[:, :, 0, :, None] + jnp.einsum('bhcd,bhcv->bhdv', kt, vt)
        return new, state

    state0 = jnp.zeros((B, H, dk, dv), jnp.float32)
    _, states = lax.scan(step, state0, (k_to_end, vc, jnp.exp(b_last)))
    o_inter = jnp.einsum('nbhid,nbhdv->nbhiv', q_dec, states)
    return (o_intra + o_inter).transpose(1, 0, 3, 2, 4).reshape(B, S, H, dv)


def gla_mixer(q, k, v, r, gz_f, gz_b, w_gate_f, b_gate_f, w_gate_b, b_gate_b, g_gla_out):
    B, S, _ = q.shape
    q = q.reshape(B, S, GLA_HEADS, GLA_DK) * (GLA_DK ** -0.5)
    k = k.reshape(B, S, GLA_HEADS, GLA_DK)
    v = v.reshape(B, S, GLA_HEADS, GLA_DV)

    def log_gate(gz, w, bias):
        z = gz.astype(jnp.float32) @ w.astype(jnp.float32) + bias.astype(jnp.float32)
        return (jax.nn.log_sigmoid(z) / GLA_TAU).reshape(B, S, GLA_HEADS, GLA_DK)

    la_f = log_gate(gz_f, w_gate_f, b_gate_f)
    la_b = log_gate(gz_b, w_gate_b, b_gate_b)
    o_f = gla_chunked(q, k, v, la_f)
    flip = lambda t: jnp.flip(t, axis=1)
    o_b = flip(gla_chunked(flip(q), flip(k), flip(v), flip(la_b)))
    o = rms_norm(o_f + o_b, g_gla_out)
    return o.reshape(B, S, GLA_WIDTH).astype(r.dtype) * jax.nn.silu(r)


def dilated_band_attention(q, k, v, dilation, half):
    B, S, H, hd = q.shape
    M = S // dilation
    N = B * dilation

    def split(t):
        return t.reshape(B, M, dilation, H, hd).transpose(0, 2, 1, 3, 4).reshape(N, M, H, hd)

    qs, ks, vs = split(q), split(k), split(v)
    Qb = DIL_BLOCK
    nb = -(-M // Qb)
    Mp = nb * Qb
    L = Qb + 2 * half
    qs = jnp.pad(qs, ((0, 0), (0, Mp - M), (0, 0), (0, 0)))
    pad_kv = ((0, 0), (half, Mp - M + half), (0, 0), (0, 0))
    ks = jnp.pad(ks, pad_kv)
    vs = jnp.pad(vs, pad_kv)
    kidx = jnp.arange(nb)[:, None] * Qb + jnp.arange(L)[None, :]
    kb = ks[:, kidx]
    vb = vs[:, kidx].astype(jnp.float32)
    qb = qs.reshape(N, nb, Qb, H, hd)
    s = jnp.einsum('nbqhd,nbkhd->nbhqk', qb, kb).astype(jnp.float32) * (hd ** -0.5)
    qpos = jnp.arange(nb)[:, None] * Qb + jnp.arange(Qb)[None, :]
    kpos = kidx - half
    rel = kpos[:, None, :] - qpos[:, :, None]
    valid = (jnp.abs(rel) <= half) & (kpos[:, None, :] >= 0) & (kpos[:, None, :] < M)
    s = jnp.where(valid[None, :, None], s, NEG_INF)
    m = jnp.max(s, axis=-1, keepdims=True)
    p = jnp.exp(s - m)
    den = jnp.sum(p, axis=-1, keepdims=True)
    o = jnp.einsum('nbhqk,nbkhd->nbqhd', p / den, vb)
    lse = (m + jnp.log(den))[..., 0].transpose(0, 1, 3, 2)
    o = o.reshape(N, Mp, H, hd)[:, :M]
    lse = lse.reshape(N, Mp, H)[:, :M]

    def merge(t):
        return t.reshape(B, dilation, M, *t.shape[2:]).swapaxes(1, 2).reshape(B, S, *t.shape[2:])

    return merge(o), merge(lse)


def dilated_mixer(q, k, v, positions):
    B, S, _ = q.shape
    q = apply_rope(q.reshape(B, S, DIL_HEADS, DIL_HD), positions)
    k = apply_rope(k.reshape(B, S, DIL_HEADS, DIL_HD), positions)
    v = v.reshape(B, S, DIL_HEADS, DIL_HD)
    outs, lses = [], []
    for window, dilation in DIL_PATTERNS:
        o, l = dilated_band_attention(q, k, v, dilation, window // (2 * dilation))
        outs.append(o)
        lses.append(l)
    w = jax.nn.softmax(jnp.stack(lses, axis=0), axis=0)
    o = jnp.sum(w[..., None] * jnp.stack(outs, axis=0), axis=0)
    return o.reshape(B, S, DIL_WIDTH).astype(q.dtype)


def peer_ffn(h, w_peer_q, peer_sub_keys, peer_u, peer_v):
    B, S, D = h.shape
    T = B * S
    x = h.reshape(T, D)
    q = (x @ w_peer_q).reshape(T, PEER_HEADS, 2, PEER_QDIM // 2)
    scores = jnp.einsum('thsd,hskd->thsk', q, peer_sub_keys).astype(jnp.float32)
    top_s, top_i = lax.top_k(scores, PEER_TOPK)
    cand_s = (top_s[:, :, 0, :, None] + top_s[:, :, 1, None, :]).reshape(T, PEER_HEADS, PEER_TOPK * PEER_TOPK)
    cand_i = (top_i[:, :, 0, :, None] * PEER_NKEYS + top_i[:, :, 1, None, :]).reshape(T, PEER_HEADS, PEER_TOPK * PEER_TOPK)
    best_s, pos = lax.top_k(cand_s, PEER_TOPK)
    idx = jnp.take_along_axis(cand_i, pos, axis=-1)
    gate = jax.nn.softmax(best_s, axis=-1)
    Tb = PEER_TOKEN_BLOCK
    nblk = T // Tb
    HK = PEER_HEADS * PEER_TOPK

    def block(args):
        xb, ib, gb = args
        a = jnp.einsum('tkd,td->tk', peer_u[ib], xb).astype(jnp.float32)
        wgt = (gb * jax.nn.gelu(a, approximate=False)).astype(xb.dtype)
        return jnp.einsum('tk,tkd->td', wgt, peer_v[ib])

    out = lax.map(block, (x.reshape(nblk, Tb, D), idx.reshape(nblk, Tb, HK), gate.reshape(nblk, Tb, HK)))
    return out.reshape(B, S, D)


def setup_inputs(seed: int = 0) -> dict:
    key = jax.random.key(seed)
    ks = jax.random.split(key, 20)
    nrm = lambda k, shape, std: jax.random.normal(k, shape, jnp.float32) * std
    L, D = DEPTH, D_MODEL
    return {
        "x": nrm(ks[0], (BATCH, SEQ, D), 1.0),
        "c": nrm(ks[1], (BATCH, D), 1.0),
        "positions": jnp.broadcast_to(jnp.arange(SEQ, dtype=jnp.int32), (BATCH, SEQ)),
        "w_ada": nrm(ks[2], (L, D, 6 * D), 0.5 * D ** -0.5),
        "b_ada": nrm(ks[3], (L, 6 * D), 0.02),
        "g_norm_mix": 1.0 + nrm(ks[4], (L, D), 0.02),
        "w_in": nrm(ks[5], (L, D, IN_WIDTH), D ** -0.5),
        "w_gate_f": nrm(ks[6], (L, GLA_GATE_RANK, GLA_HEADS * GLA_DK), GLA_GATE_RANK ** -0.5),
        "b_gate_f": nrm(ks[7], (L, GLA_HEADS * GLA_DK), 0.1),
        "w_gate_b": nrm(ks[8], (L, GLA_GATE_RANK, GLA_HEADS * GLA_DK), GLA_GATE_RANK ** -0.5),
        "b_gate_b": nrm(ks[9], (L, GLA_HEADS * GLA_DK), 0.1),
        "g_gla_out": 1.0 + nrm(ks[10], (L, GLA_HEADS, GLA_DV), 0.02),
        "w_out": nrm(ks[11], (L, MIX_WIDTH, D), MIX_WIDTH ** -0.5),
        "g_norm_ffn": 1.0 + nrm(ks[12], (L, D), 0.02),
        "w_peer_q": nrm(ks[13], (L, D, PEER_HEADS * PEER_QDIM), D ** -0.5),
        "peer_sub_keys": nrm(ks[14], (L, PEER_HEADS, 2, PEER_NKEYS, PEER_QDIM // 2), (PEER_QDIM // 2) ** -0.5),
        "peer_u": nrm(ks[15], (L, PEER_NEXPERTS, D), D ** -0.5),
        "peer_v": nrm(ks[16], (L, PEER_NEXPERTS, D), 0.5),
        "g_final": 1.0 + nrm(ks[17], (D,), 0.02),
    }


def reference(x, c, positions, w_ada, b_ada, g_norm_mix, w_in, w_gate_f, b_gate_f, w_gate_b, b_gate_b,
              g_gla_out, w_out, g_norm_ffn, w_peer_q, peer_sub_keys, peer_u, peer_v, g_final):
    for l in range(DEPTH):
        mod = jax.nn.silu(c) @ w_ada[l] + b_ada[l]
        sh_a, sc_a, ga_a, sh_f, sc_f, ga_f = [m[:, None, :] for m in jnp.split(mod, 6, axis=-1)]
        h = rms_norm(x, g_norm_mix[l]) * (1.0 + sc_a) + sh_a
        proj = h @ w_in[l]
        gq, gk, gv, gr, gz_f, gz_b, dq, dk, dv = split_cols(proj, IN_SIZES)
        o_gla = gla_mixer(gq, gk, gv, gr, gz_f, gz_b, w_gate_f[l], b_gate_f[l], w_gate_b[l], b_gate_b[l], g_gla_out[l])
        o_dil = dilated_mixer(dq, dk, dv, positions)
        mixed = jnp.concatenate([o_gla, o_dil], axis=-1) @ w_out[l]
        x = x + ga_a * mixed
        h = rms_norm(x, g_norm_ffn[l]) * (1.0 + sc_f) + sh_f
        x = x + ga_f * peer_ffn(h, w_peer_q[l], peer_sub_keys[l], peer_u[l], peer_v[l])
    return rms_norm(x, g_final)
```

```python
import os
import numpy as np
from contextlib import ExitStack
import concourse.bass as bass
import concourse.mybir as mybir
from concourse.bass_utils import run_bass_kernel_spmd

F32 = mybir.dt.float32
BF16 = mybir.dt.bfloat16
U32 = mybir.dt.uint32
I32 = mybir.dt.int32
ALU = mybir.AluOpType
AF = mybir.ActivationFunctionType
AX = mybir.AxisListType

ENGS = ['pe', 'act', 'dve', 'pool', 'sp']


class Sched:
    def __init__(self, nc, es, n_dma_sems=24):
        self.nc = nc
        self.es = es
        self.prog = {e: [] for e in ENGS}
        self.cnt = {e: 0 for e in ENGS}
        self.sems = {}
        for e in ENGS:
            self.sems[e] = es.enter_context(nc.semaphore("sem_" + e))
        self.dq = {}
        for q, qe in [('sp', 'sp'), ('pool', 'pool'), ('act', 'act'), ('bg', 'pool')]:
            pool = []
            for i in range(n_dma_sems):
                k = "dq_%s_%d" % (q, i)
                self.sems[k] = es.enter_context(nc.semaphore(k))
                pool.append(k)
            self.dq[q] = {'pool': pool, 'uses': [0] * n_dma_sems, 'next': 0, 'eng': qe}
        self.waited = {}
        self.W = {}
        self.R = {}
        self.finals = []
        self.nalloc = 0

    def sb(self, name, shape, dt, es=None):
        return (es or self.es).enter_context(self.nc.sbuf_tensor(name, list(shape), dt))

    def ps(self, name, shape, dt, es=None):
        return (es or self.es).enter_context(self.nc.psum_tensor(name, list(shape), dt))

    def _wait(self, eng, tok):
        sk, val = tok
        if self.waited.get((eng, sk), 0) >= val:
            return
        self.waited[(eng, sk)] = val
        sem = self.sems[sk]
        self.prog[eng].append(lambda e, sem=sem, val=val: e.wait_ge(sem, val))

    def _deps(self, eng, r, w):
        for n in r:
            for tok in self.W.get(n, ()):
                if tok[0] == eng and eng == 'pe':
                    continue
                self._wait(eng, tok)
            if n.startswith('ps'):
                for tok in self.R.get(n, ()):
                    if tok[0] == eng:
                        continue
                    self._wait(eng, tok)
        for n in w:
            for tok in self.W.get(n, ()):
                if tok[0] == eng and eng == 'pe':
                    continue
                self._wait(eng, tok)
            for tok in self.R.get(n, ()):
                if tok[0] == eng and eng == 'pe':
                    continue
                self._wait(eng, tok)

    def _commit(self, tok, r, w):
        for n in r:
            lst = self.R.setdefault(n, [])
            lst[:] = [t for t in lst if t[0] != tok[0]]
            lst.append(tok)
        for n in w:
            self.W[n] = [tok]
            self.R[n] = []

    def op(self, eng, fn, r=(), w=()):
        self._deps(eng, r, w)
        self.cnt[eng] += 1
        tok = (eng, self.cnt[eng])
        sem = self.sems[eng]
        self.prog[eng].append(lambda e, fn=fn, sem=sem: fn(e).then_inc(sem, 1))
        self._commit(tok, r, w)
        return tok

    def dma(self, qn, fn, r=(), w=(), final=False):
        d = self.dq[qn]
        q = d['eng']
        self._deps(q, r, w)
        i = d['next']
        d['next'] = (i + 1) % len(d['pool'])
        sk = d['pool'][i]
        if d['uses'][i] > 0:
            self._wait(q, (sk, 16 * d['uses'][i]))
        d['uses'][i] += 1
        tok = (sk, 16 * d['uses'][i])
        sem = self.sems[sk]
        self.prog[q].append(lambda e, fn=fn, sem=sem: fn(e).then_inc(sem, 16))
        self._commit(tok, r, w)
        if final:
            self.finals.append(tok)
        return tok

    def barrier(self, include_bg=False):
        toks = [(e, self.cnt[e]) for e in ENGS if self.cnt[e] > 0]
        for q, d in self.dq.items():
            if q == 'bg' and not include_bg:
                continue
            for i, sk in enumerate(d['pool']):
                if d['uses'][i] > 0:
                    toks.append((sk, 16 * d['uses'][i]))
        for e in ENGS:
            for tok in toks:
                if tok[0] == e:
                    continue
                self._wait(e, tok)
        keepW = {k: v for k, v in self.W.items() if k.startswith('bg_')}
        self.W.clear()
        self.R.clear()
        self.W.update(keepW)

    def flush(self):
        nc = self.nc
        prog = self.prog
        self.prog = {e: [] for e in ENGS}
        with nc.Block() as block:
            @block.tensor
            def _(e):
                for f in prog['pe']:
                    f(e)

            @block.scalar
            def _(e):
                for f in prog['act']:
                    f(e)

            @block.vector
            def _(e):
                for f in prog['dve']:
                    f(e)

            @block.gpsimd
            def _(e):
                for f in prog['pool']:
                    f(e)

            @block.sync
            def _(e):
                for f in prog['sp']:
                    f(e)

    def end_phase(self):
        self.barrier()
        self.flush()

    def finish(self):
        for tok in self.finals:
            self._wait('sp', tok)
        self.flush()


D = 2048
NTOK = 4096
OWN0 = 2048
EPS = 1e-6
TWO_PI = 6.283185307179586
PI = 3.141592653589793


def _consts():
    j = np.arange(128)[:, None]
    i = np.arange(128)[None, :]
    c = {}
    c["ident_f"] = np.eye(128, dtype=np.float32)
    c["LT_f"] = (j <= i).astype(np.float32)
    c["LT_b"] = (j >= i).astype(np.float32)
    c["pswap"] = (j == (i + 64) % 128).astype(np.float32)
    half = 64
    inv = np.power(10000.0, -np.arange(half, dtype=np.float32) * 2.0 / 128.0).astype(np.float32)
    c["invf"] = np.concatenate([inv, inv]).reshape(128, 1).astype(np.float32)
    sgn = np.concatenate([-np.ones(64), np.ones(64)]).reshape(128, 1).astype(np.float32)
    c["sgn"] = sgn
    mA = (j >= i).astype(np.float32)
    mB = (j <= i).astype(np.float32)
    mBl = mB * (j < 64)
    c["maskAB"] = np.concatenate([mA, mB], axis=1).astype(np.float32)
    c["maskABl"] = np.concatenate([mA, mBl], axis=1).astype(np.float32)
    c["ones_col"] = np.ones((128, 1), np.float32)
    c["iota16"] = np.tile(np.arange(16, dtype=np.float32)[None, :], (128, 1))
    return c


CONST_SHAPES = {k: v.shape for k, v in _consts().items()}


def build(phases="ABCDEFGH", dbg=()):
    nc = bass.Bass("TRN2", target_bir_lowering=False)

    def din(name, shape, dt=F32):
        return nc.dram_tensor(name, list(shape), dt, kind="ExternalInput").ap()

    def dscr(name, shape, dt):
        kind = "ExternalOutput" if name in dbg else "Internal"
        return nc.dram_tensor(name, list(shape), dt, kind=kind).ap()

    x_loc = din("x_loc", [NTOK, D])
    pos_in = din("pos", [1, NTOK], I32)
    cT_in = din("cT", [128, 16])
    w_ada = din("w_ada", [D, 6 * D])
    badaT_in = din("b_ada_row", [1, 6 * D])
    gmixT_in = din("g_mixT", [128, 16])
    gffnT_in = din("g_ffnT", [128, 16])
    gffn_row = din("g_ffn_row", [1, D])
    gfin_row = din("g_fin_row", [1, D])
    ggla_row = din("g_gla_row", [1, 1024])
    w_in = din("w_in", [D, 6176])
    w_gz = din("w_gz", [D, 32])
    Wg_in = din("Wg", [33, 1024])
    w_out = din("w_out", [D, D])
    w_pq = din("w_pq", [D, D])
    keysT_in = din("keysT", [128, 16, 128])
    peer_u = din("peer_u", [16384, D])
    peer_v = din("peer_v", [16384, D])
    cst = {k: din("c_" + k, list(s)) for k, s in CONST_SHAPES.items()}
    out_d = nc.dram_tensor("out", [2048, D], F32, kind="ExternalOutput").ap()

    mod_d = dscr("mod_d", [96, 128], F32)
    hT_d = dscr("hT_d", [32, 128, 16, 128], BF16)
    gtok_d = dscr("gtok_d", [32, 128, 3072], BF16)
    gT_d = dscr("gT_d", [16, 128, 16, 128], BF16)
    el_d = dscr("el_d", [32, 128, 8], F32)
    ob_d = dscr("ob_d", [16, 128, 1024], F32)
    cat_d = dscr("cat_d", [16, 128, 2048], BF16)
    qT_d = dscr("qT_d", [8, 128, 2048], BF16)
    kT_d = dscr("kT_d", [8, 128, 4096], BF16)
    v_d = dscr("v_d", [4096, 1032], BF16)
    oacc_d = dscr("oacc_d", [3, 2048, 1032], F32)
    x1_d = dscr("x1_d", [16, 128, D], F32)
    ub_d = dscr("ub_d", [16384, D], BF16)
    vb_d = dscr("vb_d", [16384, D], BF16)

    with ExitStack() as es:
        S = Sched(nc, es)

        def mm(out, lhsT, rhs, start, stop, r, w):
            S.op('pe', lambda e: e.matmul(out, lhsT, rhs, start=start, stop=stop), r=r, w=w)

        def tr(out, in_, ident, r, w):
            S.op('pe', lambda e: e.transpose(out, in_, ident), r=r, w=w)

        def act(out, in_, func, r, w, bias=None, scale=None, accum_out=None):
            kw = {}
            if bias is not None:
                kw['bias'] = bias
            if scale is not None:
                kw['scale'] = scale
            if accum_out is not None:
                kw['accum_out'] = accum_out
            S.op('act', lambda e: e.activation(out=out, in_=in_, func=func, **kw), r=r, w=w)

        def ts(eng, out, in0, s1, s2, op0, op1, r, w):
            if op1 is None:
                S.op(eng, lambda e: e.tensor_scalar(out=out, in0=in0, scalar1=s1, scalar2=None, op0=op0), r=r, w=w)
            else:
                S.op(eng, lambda e: e.tensor_scalar(out=out, in0=in0, scalar1=s1, scalar2=s2, op0=op0, op1=op1), r=r, w=w)

        def stt(out, in0, scalar, in1, op0, op1, r, w):
            S.op('dve', lambda e: e.scalar_tensor_tensor(out=out, in0=in0, scalar=scalar, in1=in1, op0=op0, op1=op1), r=r, w=w)

        def tt(eng, out, in0, in1, op, r, w):
            S.op(eng, lambda e: e.tensor_tensor(out=out, in0=in0, in1=in1, op=op), r=r, w=w)

        def cp(eng, out, in_, r, w):
            S.op(eng, lambda e: e.tensor_copy(out=out, in_=in_), r=r, w=w)

        def ld(out, in_, w, r=(), q='sp'):
            S.dma(q, lambda e: e.dma_start(out=out, in_=in_), r=r, w=w)

        def st(out, in_, r, w, q='sp', final=False):
            S.dma(q, lambda e: e.dma_start(out=out, in_=in_), r=r, w=w, final=final)

        def rstd_from_ss(ss, var, sd, rstd, n, tag):
            ts('dve', var, ss, 1.0 / n, EPS, ALU.mult, ALU.add, r=[tag + 'ss'], w=[tag + 'var'])
            act(sd, var, AF.Sqrt, r=[tag + 'var'], w=[tag + 'sd'])
            S.op('dve', lambda e: e.reciprocal(out=rstd, in_=sd), r=[tag + 'sd'], w=[tag + 'rstd'])

        ident_f = S.sb("ident_f", [128, 128], F32)
        ident_b = S.sb("ident_b", [128, 128], BF16)
        ld(ident_f[:], cst["ident_f"], w=['ident_f'])
        cp('dve', ident_b[:], ident_f[:], r=['ident_f'], w=['ident_b'])
        modT = S.sb("modT", [128, 96], F32)
        gscaT = S.sb("gscaT", [128, 16], F32)
        gscfT = S.sb("gscfT", [128, 16], F32)
        mod_flat = mod_d.rearrange("a b -> (a b)")

        bg_list = []
        if 'H' in phases:
            for i in range(64):
                bg_list.append((ub_d[i * 256:(i + 1) * 256, :], peer_u[i * 256:(i + 1) * 256, :], 'bg_ub%d' % i))
            for i in range(64):
                bg_list.append((vb_d[i * 256:(i + 1) * 256, :], peer_v[i * 256:(i + 1) * 256, :], 'bg_vb%d' % i))

        def bg_issue(n, dep=None):
            for _ in range(n):
                if not bg_list:
                    return
                o_, i_, nm_ = bg_list.pop(0)
                S.dma('bg', lambda e, o_=o_, i_=i_: e.dma_start(out=o_, in_=i_), r=([dep] if dep else []), w=[nm_])

        if 'A' in phases:
            with ExitStack() as pes:
                c_sb = S.sb("c_sb", [128, 16], F32, pes)
                sc_sb = S.sb("sc_sb", [128, 16], F32, pes)
                gmix_sb = S.sb("gmix_sb", [128, 16], F32, pes)
                gffn_sb = S.sb("gffn_sb", [128, 16], F32, pes)
                modrow = S.sb("modrow", [1, 6 * D], F32, pes)
                badar = S.sb("badar", [1, 6 * D], F32, pes)
                one1 = S.sb("one1", [1, 1], F32, pes)
                wa = [S.sb("wa%d" % i, [128, 16, 512], F32, pes) for i in range(2)]
                ps_row = [S.ps("ps_rowA%d" % i, [128, 512], F32, pes) for i in range(2)]
                ps_mod = S.ps("ps_mod", [128, 512], F32, pes)
                ld(c_sb[:], cT_in, w=['c_sb'])
                ld(badar[:], badaT_in, w=['badar'])
                ld(gmix_sb[:], gmixT_in, w=['gmix'])
                ld(gffn_sb[:], gffnT_in, w=['gffn'])
                S.op('dve', lambda e: e.memset(one1[:], 1.0), r=[], w=['one1'])
                act(sc_sb[:], c_sb[:], AF.Silu, r=['c_sb'], w=['sc_sb'])
                w_ada_v = w_ada.rearrange("(kc p) n -> p kc n", p=128)
                for ng in range(24):
                    b = ng % 2
                    for q4 in range(4):
                        ld(wa[b][:, q4 * 4:(q4 + 1) * 4, :], w_ada_v[:, q4 * 4:(q4 + 1) * 4, ng * 512:(ng + 1) * 512],
                           w=['wa%d_%d' % (b, q4)])
                    for kc in range(16):
                        mm(ps_row[b][0:1, :], sc_sb[:, kc:kc + 1], wa[b][:, kc, :], kc == 0, kc == 15,
                           r=['wa%d_%d' % (b, kc // 4), 'sc_sb'], w=['ps_rowA%d' % b])
                    tt('dve', modrow[0:1, ng * 512:(ng + 1) * 512], ps_row[b][0:1, :], badar[0:1, ng * 512:(ng + 1) * 512], ALU.add,
                       r=['ps_rowA%d' % b, 'badar'], w=['modrow%d' % ng])
                mr_all = ['modrow%d' % ng for ng in range(24)]
                st(mod_d.rearrange("a b -> (a b)").rearrange("(o n) -> o n", o=1), modrow[:], r=mr_all, w=['mod_d'])
                for c in range(96):
                    mm(ps_mod[:, c:c + 1], modrow[0:1, c * 128:(c + 1) * 128], one1[0:1, 0:1], True, True,
                       r=mr_all + ['one1'], w=['ps_mod'])
                cp('dve', modT[:], ps_mod[:, 0:96], r=['ps_mod'], w=['modT'])
                stt(gscaT[:], modT[:, 16:32], 1.0, gmix_sb[:], ALU.add, ALU.mult, r=['modT', 'gmix'], w=['gscaT'])
                stt(gscfT[:], modT[:, 64:80], 1.0, gffn_sb[:], ALU.add, ALU.mult, r=['modT', 'gffn'], w=['gscfT'])
                S.end_phase()

        ones_col = S.sb("ones_col", [128, 1], F32)
        ld(ones_col[:], cst["ones_col"], w=['ones_col'])
        pre_wg = ExitStack()
        wg = S.sb("wg", [128, 16, 3104], BF16, pre_wg)
        if 'C' in phases:
            for kc in range(16):
                for pc in range(3):
                    ld(wg[:, kc, pc * 1024:(pc + 1) * 1024], w_in[kc * 128:(kc + 1) * 128, pc * 1024:(pc + 1) * 1024],
                       w=['wg%d_%d' % (kc, pc)], q='pool')
                ld(wg[:, kc, 3072:3104], w_gz[kc * 128:(kc + 1) * 128, :], w=['wg%d_3' % kc], q='pool')
        if 'B' in phases:
            with ExitStack() as pes:
                xt = [S.sb("xt%d" % i, [128, D], F32, pes) for i in range(2)]
                xn = [S.sb("xn%d" % i, [128, D], BF16, pes) for i in range(2)]
                hs = [S.sb("hs%d" % i, [128, 16, 128], BF16, pes) for i in range(2)]
                junk = S.sb("junkB", [128, D], BF16, pes)
                st4 = [S.sb("st4_%d" % i, [128, 4], F32, pes) for i in range(2)]
                ps_t = [[S.ps("ps_tB%d_%d" % (i, hf), [128, 1024], BF16, pes) for hf in range(2)] for i in range(2)]
                def b_stage1(t):
                    b = t % 2
                    B = str(b)
                    ld(xt[b][:], x_loc[t * 128:(t + 1) * 128, :], w=['xt' + B])
                    act(junk[:], xt[b][:], AF.Square, r=['xt' + B], w=['junkB', 'B%sss' % B], accum_out=st4[b][:, 0:1])
                    rstd_from_ss(st4[b][:, 0:1], st4[b][:, 1:2], st4[b][:, 2:3], st4[b][:, 3:4], D, 'B' + B)
                    act(xn[b][:], xt[b][:], AF.Copy, r=['xt' + B, 'B%srstd' % B], w=['xn' + B], scale=st4[b][:, 3:4])

                def b_stage2(t):
                    b = t % 2
                    B = str(b)
                    for kc in range(16):
                        hf, k8 = kc // 8, kc % 8
                        tr(ps_t[b][hf][:, k8 * 128:(k8 + 1) * 128], xn[b][:, kc * 128:(kc + 1) * 128], ident_b[:],
                           r=['xn' + B, 'ident_b'], w=['ps_tB%s_%d' % (B, hf)])
                    for kc in range(16):
                        hf, k8 = kc // 8, kc % 8
                        if hf == 0:
                            ts('dve', hs[b][:, kc, :], ps_t[b][hf][:, k8 * 128:(k8 + 1) * 128], gscaT[:, kc:kc + 1],
                               modT[:, kc:kc + 1], ALU.mult, ALU.add,
                               r=['ps_tB%s_%d' % (B, hf), 'gscaT', 'modT'], w=['hs%s_%d' % (B, kc)])
                        else:
                            act(hs[b][:, kc, :], ps_t[b][hf][:, k8 * 128:(k8 + 1) * 128], AF.Identity,
                                r=['ps_tB%s_%d' % (B, hf), 'gscaT', 'modT'], w=['hs%s_%d' % (B, kc)],
                                bias=modT[:, kc:kc + 1], scale=gscaT[:, kc:kc + 1])
                    st(hT_d[t], hs[b][:], r=['hs%s_%d' % (B, kc) for kc in range(16)], w=['hT_d%d' % t])

                b_stage1(0)
                for t in range(32):
                    if t + 1 < 32:
                        b_stage1(t + 1)
                    b_stage2(t)
                    bg_issue(2, 'xn%d' % (t % 2))
                S.end_phase()

        if 'C' in phases:
            with ExitStack() as pes:
                Wg_sb = S.sb("Wg_sb", [33, 1024], F32, pes)
                LTf = S.sb("LTf", [128, 128], F32, pes)
                LTb = S.sb("LTb", [128, 128], F32, pes)
                gz_sb = S.sb("gz_sb", [33, 128], F32, pes)
                hTt = [S.sb("hTt%d" % i, [128, 16, 128], BF16, pes) for i in range(2)]
                e1 = S.sb("e1", [128, 1024], F32, pes)
                l1 = S.sb("l1", [128, 1024], F32, pes)
                epos = S.sb("epos", [128, 1024], F32, pes)
                eneg = S.sb("eneg", [128, 1024], F32, pes)
                elx = [S.sb("elx%d" % i, [128, 8], F32, pes) for i in range(2)]
                qd = S.sb("qd", [128, 1024], BF16, pes)
                tk = [S.sb("tk%d" % i, [128, 3072], BF16, pes) for i in range(2)]
                tT = [S.sb("tT%d" % i, [128, 16, 128], BF16, pes) for i in range(2)]
                ps_p = [S.ps("ps_pC%d" % i, [128, 512], F32, pes) for i in range(3)]
                ps_gz = S.ps("ps_gzC", [128, 512], F32, pes)
                ps_z = [S.ps("ps_zC%d" % i, [128, 512], F32, pes) for i in range(2)]
                ps_tr = [S.ps("ps_trC%d" % i, [128, 1024], BF16, pes) for i in range(2)]
                ld(Wg_sb[:], Wg_in, w=['Wg_sb'])
                ld(LTf[:], cst["LT_f"], w=['LTf'])
                ld(LTb[:], cst["LT_b"], w=['LTb'])
                S.op('dve', lambda e: e.memset(gz_sb[:], 1.0), r=[], w=['gz_sb'])
                wg_all = ['wg%d_%d' % (kc, pc) for kc in range(16) for pc in range(4)]
                pcnt = [0]

                def proj_chunk(b, chunk):
                    pb = pcnt[0] % 3
                    pcnt[0] += 1
                    for kc in range(16):
                        mm(ps_p[pb][:, :], hTt[b][:, kc, :], wg[:, kc, chunk * 512:(chunk + 1) * 512], kc == 0, kc == 15,
                           r=['hTt%d' % b] + wg_all, w=['ps_pC%d' % pb])
                    return pb

                ld(hTt[0][:], hT_d[0], r=['hT_d0'], w=['hTt0'])
                for t in range(32):
                    own = t >= 16
                    b = t % 2
                    if t + 1 < 32:
                        ld(hTt[1 - b][:], hT_d[t + 1], r=['hT_d%d' % (t + 1)], w=['hTt%d' % (1 - b)])
                    for kc in range(16):
                        mm(ps_gz[0:32, 0:128], wg[:, kc, 3072:3104], hTt[b][:, kc, :], kc == 0, kc == 15,
                           r=['hTt%d' % b] + wg_all, w=['ps_gzC'])
                    act(gz_sb[0:32, :], ps_gz[0:32, 0:128], AF.Copy, r=['ps_gzC'], w=['gz_sb'])
                    pv0 = proj_chunk(b, 2)
                    for hf in range(2):
                        mm(ps_z[hf][:, :], gz_sb[:, :], Wg_sb[:, hf * 512:(hf + 1) * 512], True, True,
                           r=['gz_sb', 'Wg_sb'], w=['ps_zC%d' % hf])
                    for hf in range(2):
                        act(e1[:, hf * 512:(hf + 1) * 512], ps_z[hf][:, :], AF.Exp, r=['ps_zC%d' % hf], w=['e1_%d' % hf], scale=-1.0)
                    act(l1[:], e1[:], AF.Ln, r=['e1_0', 'e1_1', 'ones_col'], w=['l1'], bias=ones_col[:, 0:1], scale=1.0)
                    act(tk[b][:, 1024:1536], ps_p[pv0][:, :], AF.Copy, r=['ps_pC%d' % pv0], w=['tk%d_v0' % b])
                    pv1 = proj_chunk(b, 3)
                    act(tk[b][:, 1536:2048], ps_p[pv1][:, :], AF.Copy, r=['ps_pC%d' % pv1], w=['tk%d_v1' % b])
                    mm(ps_z[0][:, :], LTf[:], l1[:, 0:512], True, True, r=['LTf', 'l1'], w=['ps_zC0'])
                    mm(ps_z[1][:, :], LTb[:], l1[:, 512:1024], True, True, r=['LTb', 'l1'], w=['ps_zC1'])
                    ps_elv = ps_tr[0][:, :].bitcast(F32)
                    for c in range(8):
                        mm(ps_elv[:, c:c + 1], l1[:, c * 128:(c + 1) * 128], ones_col[:, 0:1], True, True,
                           r=['l1', 'ones_col'], w=['ps_trC0'])
                    pk = proj_chunk(b, 1)
                    for hf in range(2):
                        if own or hf == 0:
                            act(eneg[:, hf * 512:(hf + 1) * 512], ps_z[hf][:, :], AF.Exp, r=['ps_zC%d' % hf], w=['eneg%d' % hf], scale=1.0 / 16)
                        if own:
                            act(epos[:, hf * 512:(hf + 1) * 512], ps_z[hf][:, :], AF.Exp, r=['ps_zC%d' % hf], w=['epos%d' % hf], scale=-1.0 / 16)
                    act(elx[b][:], ps_elv[:, 0:8], AF.Exp, r=['ps_trC0'], w=['elx%d' % b], scale=-1.0 / 16)
                    st(el_d[t], elx[b][:], r=['elx%d' % b], w=['el_d%d' % t])
                    tt('dve', tk[b][:, 0:512], ps_p[pk][:, :], eneg[:, 0:512], ALU.mult, r=['ps_pC%d' % pk, 'eneg0'], w=['tk%d_kf' % b])
                    bg_issue(2, 'tk%d_kf' % b)
                    if own:
                        tt('dve', tk[b][:, 512:1024], ps_p[pk][:, :], eneg[:, 512:1024], ALU.mult, r=['ps_pC%d' % pk, 'eneg1'], w=['tk%d_kb' % b])
                        pq = proj_chunk(b, 0)
                        stt(qd[:, 0:512], ps_p[pq][:, :], 128.0 ** -0.5, epos[:, 0:512], ALU.mult, ALU.mult,
                            r=['ps_pC%d' % pq, 'epos0'], w=['qd0'])
                        stt(qd[:, 512:1024], ps_p[pq][:, :], 128.0 ** -0.5, epos[:, 512:1024], ALU.mult, ALU.mult,
                            r=['ps_pC%d' % pq, 'epos1'], w=['qd1'])
                        for c in range(2):
                            pr = proj_chunk(b, 4 + c)
                            act(tk[b][:, 2048 + c * 512:2048 + (c + 1) * 512], ps_p[pr][:, :], AF.Silu, r=['ps_pC%d' % pr], w=['tk%d_r%d' % (b, c)])
                        for dr in range(2):
                            for kind in range(2):
                                for h in range(4):
                                    k8 = kind * 4 + h
                                    if kind == 0:
                                        srcap = qd[:, dr * 512 + h * 128: dr * 512 + (h + 1) * 128]
                                        rn = 'qd%d' % dr
                                    else:
                                        srcap = tk[b][:, dr * 512 + h * 128: dr * 512 + (h + 1) * 128]
                                        rn = 'tk%d_k%s' % (b, 'fb'[dr])
                                    tr(ps_tr[dr][:, k8 * 128:(k8 + 1) * 128], srcap, ident_b[:], r=[rn, 'ident_b'], w=['ps_trC%d' % dr])
                        cp('dve', tT[b][:, 0:8, :], ps_tr[0][:, :].rearrange("p (a b) -> p a b", a=8), r=['ps_trC0'], w=['tT%d_0' % b])
                        act(tT[b][:, 8:16, :], ps_tr[1][:, :].rearrange("p (a b) -> p a b", a=8), AF.Copy, r=['ps_trC1'], w=['tT%d_1' % b])
                        st(gT_d[t - 16], tT[b][:], r=['tT%d_0' % b, 'tT%d_1' % b], w=['gT_d%d' % (t - 16)])
                        st(gtok_d[t], tk[b][:], r=['tk%d_kf' % b, 'tk%d_kb' % b, 'tk%d_v0' % b, 'tk%d_v1' % b, 'tk%d_r0' % b, 'tk%d_r1' % b],
                           w=['gtok_d%d' % t])
                    else:
                        st(gtok_d[t][:, 0:512], tk[b][:, 0:512], r=['tk%d_kf' % b], w=['gtok_d%d_a' % t])
                        st(gtok_d[t][:, 1024:2048], tk[b][:, 1024:2048], r=['tk%d_v0' % b, 'tk%d_v1' % b], w=['gtok_d%d_b' % t])
                S.end_phase()

        pre_wg.close()
        pre_wd = ExitStack()
        wd = S.sb("wd", [128, 16, 3072], BF16, pre_wd)
        if 'E' in phases:
            for kc in range(16):
                for pc in range(3):
                    ld(wd[:, kc, pc * 1024:(pc + 1) * 1024], w_in[kc * 128:(kc + 1) * 128, 3104 + pc * 1024:3104 + (pc + 1) * 1024],
                       w=['wd%d_%d' % (kc, pc)], q='pool')
        if 'D' in phases:
            with ExitStack() as pes:
                Sf = S.sb("Sf", [128, 4, 256], F32, pes)
                Sb = S.sb("Sb", [128, 4, 256], BF16, pes)
                t1 = [S.sb("t1D%d" % i, [128, 256], F32, pes) for i in range(2)]
                mask = [S.sb("maskD%d" % i, [128, 128], F32, pes) for i in range(2)]
                gglab = S.sb("gglab", [128, 1024], F32, pes)
                tkd = [S.sb("tkd%d" % i, [128, 3072], BF16, pes) for i in range(2)]
                tTd = [S.sb("tTd%d" % i, [128, 8, 128], BF16, pes) for i in range(2)]
                eld = [S.sb("eld%d" % i, [128, 8], F32, pes) for i in range(2)]
                am = [S.sb("amD%d" % i, [128, 128], BF16, pes) for i in range(2)]
                obs = [S.sb("obs%d" % i, [128, 1024], F32, pes) for i in range(2)]
                osum = S.sb("osumD", [128, 1024], F32, pes)
                og = S.sb("ogD", [128, 1024], F32, pes)
                catg = [S.sb("catg%d" % i, [128, 1024], BF16, pes) for i in range(2)]
                junk = S.sb("junkD", [128, 256], F32, pes)
                st8 = S.sb("st8D", [128, 16], F32, pes)
                ps_at = [S.ps("ps_atD%d" % i, [128, 512], F32, pes) for i in range(2)]
                ps_o = [[S.ps("ps_oD%d_%d" % (i, hf), [128, 512], F32, pes) for hf in range(2)] for i in range(2)]
                ps_s = [S.ps("ps_sD%d" % i, [128, 512], F32, pes) for i in range(2)]
                ld(mask[0][:], cst["LT_f"], w=['maskD0'])
                ld(mask[1][:], cst["LT_b"], w=['maskD1'])
                ld(gglab[:], ggla_row[0, :].partition_broadcast(128), w=['gglab'])
                cnt = [0]

                lcnt = [0]

                def gla_load(t, dr, full):
                    i = lcnt[0]
                    lcnt[0] += 1
                    b = i % 2
                    kcol = dr * 512
                    ld(tkd[b][:, kcol:kcol + 512], gtok_d[t][:, kcol:kcol + 512], w=['tkd%d_k' % b])
                    ld(tkd[b][:, 1024:2048], gtok_d[t][:, 1024:2048], w=['tkd%d_v' % b])
                    ld(eld[b][:], el_d[t], w=['eld%d' % b])
                    if full:
                        ld(tTd[b][:], gT_d[t - 16][:, dr * 8:(dr + 1) * 8, :], w=['tTd%d' % b])
                        if dr == 0:
                            ld(tkd[b][:, 2048:3072], gtok_d[t][:, 2048:3072], w=['tkd%d_r' % b])
                            ld(obs[b][:], ob_d[t - 16], r=['ob_d%d' % (t - 16)], w=['obs%d' % b])

                def gla_tile(t, dr, full):
                    i = cnt[0]
                    cnt[0] += 1
                    b = i % 2
                    kcol = dr * 512
                    def head_at(h):
                        pa = (i * 4 + h) % 2
                        mm(ps_at[pa][:, 0:128], tTd[b][:, 4 + h, :], tTd[b][:, h, :], True, True,
                           r=['tTd%d' % b], w=['ps_atD%d' % pa])
                        return pa

                    def head_mid(h, pa):
                        vh = tkd[b][:, 1024 + h * 256:1024 + (h + 1) * 256]
                        hf = h // 2
                        ocol = (h % 2) * 256
                        if full:
                            tt('dve', am[pa][:], ps_at[pa][:, 0:128], mask[dr][:], ALU.mult, r=['ps_atD%d' % pa, 'maskD%d' % dr], w=['amD%d' % pa])
                            mm(ps_o[b][hf][:, ocol:ocol + 256], am[pa][:], vh, True, False,
                               r=['amD%d' % pa, 'tkd%d_v' % b], w=['ps_oD%d_%d' % (b, hf)])
                            mm(ps_o[b][hf][:, ocol:ocol + 256], tTd[b][:, h, :], Sb[:, h, :], False, True,
                               r=['tTd%d' % b, 'Sb%d' % h], w=['ps_oD%d_%d' % (b, hf)])
                        pb = (i * 4 + h) % 2
                        mm(ps_s[pb][:, 0:256], tkd[b][:, kcol + h * 128:kcol + (h + 1) * 128], vh, True, True,
                           r=['tkd%d_k' % b, 'tkd%d_v' % b], w=['ps_sD%d' % pb])

                    def head_upd(h):
                        elh = eld[b][:, dr * 4 + h: dr * 4 + h + 1]
                        pb = (i * 4 + h) % 2
                        ts('dve', t1[h % 2][:], Sf[:, h, :], elh, None, ALU.mult, None, r=['Sf%d' % h, 'eld%d' % b], w=['t1D%d' % (h % 2)])
                        stt(Sf[:, h, :], ps_s[pb][:, 0:256], elh, t1[h % 2][:], ALU.mult, ALU.add,
                            r=['ps_sD%d' % pb, 'eld%d' % b, 't1D%d' % (h % 2)], w=['Sf%d' % h])
                        act(Sb[:, h, :], Sf[:, h, :], AF.Copy, r=['Sf%d' % h], w=['Sb%d' % h])

                    pas = [None] * 4
                    if full:
                        pas[0] = head_at(0)
                        pas[1] = head_at(1)
                    for h in range(4):
                        head_mid(h, pas[h])
                        if full and h + 2 < 4:
                            pas[h + 2] = head_at(h + 2)
                        if h >= 1:
                            head_upd(h - 1)
                    head_upd(3)
                    if not full:
                        return
                    if dr == 1:
                        for hf in range(2):
                            if hf == 0:
                                cp('dve', obs[b][:, 0:512], ps_o[b][0][:, :], r=['ps_oD%d_0' % b], w=['obs%d_0' % b])
                            else:
                                act(obs[b][:, 512:1024], ps_o[b][1][:, :], AF.Copy, r=['ps_oD%d_1' % b], w=['obs%d_1' % b])
                        st(ob_d[t - 16], obs[b][:], r=['obs%d_0' % b, 'obs%d_1' % b], w=['ob_d%d' % (t - 16)])
                        return
                    for hf in range(2):
                        tt('dve', osum[:, hf * 512:(hf + 1) * 512], ps_o[b][hf][:, :], obs[b][:, hf * 512:(hf + 1) * 512], ALU.add,
                           r=['ps_oD%d_%d' % (b, hf), 'obs%d' % b], w=['osum%d' % hf])
                    for h in range(4):
                        act(junk[:], osum[:, h * 256:(h + 1) * 256], AF.Square, r=['osum%d' % (h // 2)], w=['junkD', 'Dss%d' % h],
                            accum_out=st8[:, h:h + 1])
                    ts('dve', st8[:, 4:8], st8[:, 0:4], 1.0 / 256, EPS, ALU.mult, ALU.add, r=['Dss%d' % h for h in range(4)], w=['Dvar'])
                    act(st8[:, 8:12], st8[:, 4:8], AF.Sqrt, r=['Dvar'], w=['Dsd'])
                    S.op('dve', lambda e: e.reciprocal(out=st8[:, 12:16], in_=st8[:, 8:12]), r=['Dsd'], w=['Drstd'])
                    for h in range(4):
                        stt(og[:, h * 256:(h + 1) * 256], osum[:, h * 256:(h + 1) * 256], st8[:, 12 + h:13 + h],
                            gglab[:, h * 256:(h + 1) * 256], ALU.mult, ALU.mult,
                            r=['osum%d' % (h // 2), 'Drstd', 'gglab'], w=['og%d' % h])
                    tt('pool', catg[b][:], og[:], tkd[b][:, 2048:3072], ALU.mult, r=['og%d' % h for h in range(4)] + ['tkd%d_r' % b],
                       w=['catg%d' % b])
                    st(cat_d[t - 16][:, 0:1024], catg[b][:], r=['catg%d' % b], w=['cat_d%d_g' % (t - 16)])

                def reset_state():
                    S.op('dve', lambda e: e.memset(Sf[:], 0.0), r=[], w=['Sf%d' % h for h in range(4)])
                    S.op('pool', lambda e: e.memset(Sb[:], 0.0), r=[], w=['Sb%d' % h for h in range(4)])

                seq = [(t, 1, True) for t in range(31, 15, -1)] + [(t, 0, t >= 16) for t in range(32)]
                reset_state()
                gla_load(*seq[0])
                for n, item in enumerate(seq):
                    if n == 16:
                        reset_state()
                    if n + 1 < len(seq):
                        gla_load(*seq[n + 1])
                    gla_tile(*item)
                S.end_phase()

        bg_issue(1000)
        if 'E' in phases:
            with ExitStack() as pes:
                cosT = S.sb("cosT", [128, 3072], F32, pes)
                sinS = S.sb("sinS", [128, 3072], F32, pes)
                with ExitStack() as pes2:
                    posi = S.sb("posi", [128, 3072], I32, pes2)
                    ang = S.sb("ang", [128, 3072], F32, pes2)
                    kq = S.sb("kq", [128, 3072], I32, pes2)
                    rr_ = S.sb("rr_", [128, 3072], F32, pes2)
                    tm = S.sb("tm", [128, 3072], F32, pes2)
                    invf = S.sb("invf", [128, 1], F32, pes2)
                    sgn = S.sb("sgn", [128, 1], F32, pes2)
                    ld(invf[:], cst["invf"], w=['invf'])
                    ld(sgn[:], cst["sgn"], w=['sgn'])
                    ld(posi[:], pos_in[0, 1024:4096].partition_broadcast(128), w=['posi'])
                    cp('dve', tm[:], posi[:], r=['posi'], w=['tm'])
                    ts('dve', ang[:], tm[:], invf[:, 0:1], None, ALU.mult, None, r=['tm', 'invf'], w=['ang'])
                    ts('dve', tm[:], ang[:], 1.0 / TWO_PI, None, ALU.mult, None, r=['ang'], w=['tm'])
                    cp('dve', kq[:], tm[:], r=['tm'], w=['kq'])
                    cp('dve', tm[:], kq[:], r=['kq'], w=['tm'])
                    stt(rr_[:], tm[:], -TWO_PI, ang[:], ALU.mult, ALU.add, r=['tm', 'ang'], w=['rr_'])

                    def wrap(buf, name):
                        ts('dve', tm[:], buf[:], PI, -TWO_PI, ALU.is_gt, ALU.mult, r=[name], w=['tm'])
                        tt('dve', buf[:], buf[:], tm[:], ALU.add, r=[name, 'tm'], w=[name])
                        ts('dve', tm[:], buf[:], -PI, TWO_PI, ALU.is_lt, ALU.mult, r=[name], w=['tm'])
                        tt('dve', buf[:], buf[:], tm[:], ALU.add, r=[name, 'tm'], w=[name])
                        ts('dve', buf[:], buf[:], -PI, PI, ALU.max, ALU.min, r=[name], w=[name])

                    wrap(rr_, 'rr_')
                    act(sinS[:], rr_[:], AF.Sin, r=['rr_'], w=['sinS'])
                    ts('dve', sinS[:], sinS[:], sgn[:, 0:1], None, ALU.mult, None, r=['sinS', 'sgn'], w=['sinS'])
                    ts('dve', ang[:], rr_[:], PI / 2, None, ALU.add, None, r=['rr_'], w=['ang'])
                    wrap(ang, 'ang')
                    act(cosT[:], ang[:], AF.Sin, r=['ang'], w=['cosT'])
                    S.end_phase()
                pswap = S.sb("pswap", [128, 128], BF16, pes)
                pswf = S.sb("pswf", [128, 128], F32, pes)
                hblk = [S.sb("hblk%d" % i, [128, 4, 16, 128], BF16, pes) for i in range(2)]
                qb = [S.sb("qbE%d" % i, [128, 512], BF16, pes) for i in range(2)]
                r1 = [S.sb("r1E%d" % i, [128, 512], F32, pes) for i in range(2)]
                r2 = [S.sb("r2E%d" % i, [128, 512], F32, pes) for i in range(2)]
                rot = [S.sb("rotE%d" % i, [128, 512], BF16, pes) for i in range(2)]
                vst = [S.sb("vstE%d" % i, [128, 8, 129], BF16, pes) for i in range(2)]
                zt = S.sb("ztE", [128, 1032], BF16, pes)
                ps_p = [S.ps("ps_pE%d" % i, [128, 512], F32, pes) for i in range(2)]
                ps_w = [S.ps("ps_wE%d" % i, [128, 512], F32, pes) for i in range(2)]
                ps_v = [S.ps("ps_vE%d" % i, [128, 512], F32, pes) for i in range(2)]
                ld(pswf[:], cst["pswap"], w=['pswf'])
                cp('dve', pswap[:], pswf[:], r=['pswf'], w=['pswap'])
                S.op('pool', lambda e: e.memset(zt[:], 0.0), r=[], w=['ztE'])
                for i in range(2):
                    S.op('pool', lambda e, i=i: e.memset(vst[i][:], 1.0), r=[], w=['vstE%d' % i])
                for h in range(8):
                    st(kT_d[h][:, 3072:4096], zt[:, 0:1024], r=['ztE'], w=['kT_d_pad%d' % h])
                    st(v_d[3072 + h * 128:3072 + (h + 1) * 128, :], zt[:], r=['ztE'], w=['v_d_pad%d' % h])
                wd_all = ['wd%d_%d' % (kc, pc) for kc in range(16) for pc in range(3)]
                ucnt = [0]
                pendE = [None]
                for bk in range(6):
                    own = bk >= 2
                    b = bk % 2
                    for tl in range(4):
                        t = 8 + bk * 4 + tl
                        ld(hblk[b][:, tl], hT_d[t], r=['hT_d%d' % t], w=['hblk%d_%d' % (b, tl)])
                    hb_all = ['hblk%d_%d' % (b, tl) for tl in range(4)]
                    for h in range(8):
                        for which in ([1, 0] if own else [1]):
                            u = ucnt[0]
                            ucnt[0] += 1
                            pb = u % 2
                            cols = which * 1024 + h * 128
                            for kc in range(16):
                                mm(ps_p[pb][:, :], wd[:, kc, cols:cols + 128], hblk[b][:, :, kc, :], kc == 0, kc == 15,
                                   r=hb_all + wd_all, w=['ps_pE%d' % pb])
                            act(qb[pb][:], ps_p[pb][:, :], AF.Copy, r=['ps_pE%d' % pb], w=['qbE%d' % pb])
                            if pendE[0] is not None:
                                pendE[0]()

                            def rest(pb=pb, bk=bk, h=h, which=which):
                                mm(ps_w[pb][:, :], pswap[:], qb[pb][:], True, True, r=['pswap', 'qbE%d' % pb], w=['ps_wE%d' % pb])
                                tsl = slice(bk * 512, (bk + 1) * 512)
                                tt('dve', r1[pb][:], ps_p[pb][:, :], cosT[:, tsl], ALU.mult, r=['ps_pE%d' % pb, 'cosT'], w=['r1E%d' % pb])
                                tt('dve', r2[pb][:], ps_w[pb][:, :], sinS[:, tsl], ALU.mult, r=['ps_wE%d' % pb, 'sinS'], w=['r2E%d' % pb])
                                tt('pool', rot[pb][:], r1[pb][:], r2[pb][:], ALU.add, r=['r1E%d' % pb, 'r2E%d' % pb], w=['rotE%d' % pb])
                                if which == 1:
                                    st(kT_d[h][:, bk * 512:(bk + 1) * 512], rot[pb][:], r=['rotE%d' % pb], w=['kT_d%d_%d' % (h, bk)])
                                else:
                                    st(qT_d[h][:, (bk - 2) * 512:(bk - 1) * 512], rot[pb][:], r=['rotE%d' % pb], w=['qT_d%d_%d' % (h, bk)])
                            pendE[0] = rest
                    if pendE[0] is not None:
                        pendE[0]()
                        pendE[0] = None
                    for tl in range(4):
                        vb = (bk * 4 + tl) % 2
                        for c in range(2):
                            for kc in range(16):
                                mm(ps_v[c][:, :], hblk[b][:, tl, kc, :], wd[:, kc, 2048 + c * 512:2048 + (c + 1) * 512], kc == 0, kc == 15,
                                   r=hb_all + wd_all, w=['ps_vE%d' % c])
                            act(vst[vb][:, c * 4:(c + 1) * 4, 0:128], ps_v[c][:, :].rearrange("p (a b) -> p a b", a=4), AF.Copy,
                                r=['ps_vE%d' % c], w=['vstE%d_%d' % (vb, c)])
                        row0 = bk * 512 + tl * 128
                        st(v_d[row0:row0 + 128, :], vst[vb][:].rearrange("p a b -> p (a b)"),
                           r=['vstE%d' % vb, 'vstE%d_0' % vb, 'vstE%d_1' % vb], w=['v_d%d' % (bk * 4 + tl)])
                S.end_phase()

        pre_wd.close()
        pre_wo = ExitStack()
        wo = S.sb("wo", [128, 16, 2048], BF16, pre_wo)
        if 'G' in phases:
            for kc in range(16):
                for pc in range(2):
                    ld(wo[:, kc, pc * 1024:(pc + 1) * 1024], w_out[kc * 128:(kc + 1) * 128, pc * 1024:(pc + 1) * 1024],
                       w=['wo%d_%d' % (kc, pc)], q='pool')
        if 'F' in phases:
            with ExitStack() as pes:
                qTa = S.sb("qTa", [128, 8, 2048], BF16, pes)
                kTa = S.sb("kTa", [128, 8, 4096], BF16, pes)
                mf = S.sb("mfF", [128, 512], F32, pes)
                mAB = S.sb("mAB", [128, 256], BF16, pes)
                mABl = S.sb("mABl", [128, 256], BF16, pes)
                vA = [S.sb("vA%d" % i, [128, 1032], BF16, pes) for i in range(2)]
                vB = [S.sb("vB%d" % i, [128, 1032], BF16, pes) for i in range(2)]
                pexp = [S.sb("pexp%d" % i, [128, 256], BF16, pes) for i in range(4)]
                pm = [S.sb("pmF%d" % i, [128, 256], BF16, pes) for i in range(4)]
                ost = [S.sb("ostF%d" % i, [128, 1032], F32, pes) for i in range(2)]
                ps_s = [S.ps("ps_sF%d" % i, [128, 512], F32, pes) for i in range(4)]
                ps_o = [S.ps("ps_oF%d" % i, [128, 512], F32, pes) for i in range(4)]
                ld(mf[:, 0:256], cst["maskAB"], w=['mf0'])
                ld(mf[:, 256:512], cst["maskABl"], w=['mf1'])
                cp('dve', mAB[:], mf[:, 0:256], r=['mf0'], w=['mAB'])
                cp('dve', mABl[:], mf[:, 256:512], r=['mf1'], w=['mABl'])
                for h in range(8):
                    ld(qTa[:, h, :], qT_d[h], w=['qTa%d' % h])
                    ld(kTa[:, h, :], kT_d[h], w=['kTa%d' % h])
                bcnt = [0]
                ucnt = [0]
                pending = [None]

                def s_stage(b, h, kA, kB, DD, u0):
                    u = ucnt[0]
                    ucnt[0] += 1
                    pb = u % 4
                    qap = qTa[:, h, u0:u0 + 127 * DD + 1:DD]
                    mm(ps_s[pb][:, 0:128], kTa[:, h, kA:kA + 127 * DD + 1:DD], qap, True, True,
                       r=['qTa%d' % h, 'kTa%d' % h], w=['ps_sF%d' % pb])
                    mm(ps_s[pb][:, 128:256], kTa[:, h, kB:kB + 127 * DD + 1:DD], qap, True, True,
                       r=['qTa%d' % h, 'kTa%d' % h], w=['ps_sF%d' % pb])
                    return pb

                def o_stage(b, h, pb, msk, mname):
                    act(pexp[pb][:], ps_s[pb][:, 0:256], AF.Exp, r=['ps_sF%d' % pb], w=['pexp%d' % pb], scale=128.0 ** -0.5)
                    tt('dve', pm[pb][:], pexp[pb][:], msk[:], ALU.mult, r=['pexp%d' % pb, mname], w=['pmF%d' % pb])
                    mm(ps_o[pb][:, 0:129], pm[pb][:, 0:128], vA[b][:, h * 129:(h + 1) * 129], True, False,
                       r=['pmF%d' % pb, 'vA%d' % b], w=['ps_oF%d' % pb])
                    mm(ps_o[pb][:, 0:129], pm[pb][:, 128:256], vB[b][:, h * 129:(h + 1) * 129], False, True,
                       r=['pmF%d' % pb, 'vB%d' % b], w=['ps_oF%d' % pb])

                def c_stage(b, h, pb):
                    act(ost[b][:, h * 129:(h + 1) * 129], ps_o[pb][:, 0:129], AF.Copy, r=['ps_oF%d' % pb], w=['ostF%d_%d' % (b, h)])

                blocks = []
                for pi, DD in enumerate([1, 4, 16]):
                    nsp = 16 // DD
                    for sp_i in range(nsp):
                        for rs in range(DD):
                            u0 = 128 * DD * sp_i + rs
                            kA = 1024 + u0 - 64 * DD
                            blocks.append((pi, DD, u0, kA, kA + 128 * DD, sp_i == nsp - 1))

                def f_load(bi):
                    pi, DD, u0, kA, kB, last = blocks[bi]
                    b = bi % 2
                    ld(vA[b][:], v_d[kA:kA + 127 * DD + 1:DD, :], w=['vA%d' % b])
                    ld(vB[b][:], v_d[kB:kB + 127 * DD + 1:DD, :], w=['vB%d' % b])

                pend = []
                pendc = []
                f_load(0)
                for bi, (pi, DD, u0, kA, kB, last) in enumerate(blocks):
                    b = bi % 2
                    msk, mname = (mABl, 'mABl') if last else (mAB, 'mAB')
                    for h in range(8):
                        pb = s_stage(b, h, kA, kB, DD, u0)
                        pend.append(lambda b=b, h=h, pb=pb, msk=msk, mname=mname: o_stage(b, h, pb, msk, mname))
                        pendc.append(lambda b=b, h=h, pb=pb, pi=pi, u0=u0, DD=DD, bi=bi:
                                     (c_stage(b, h, pb),
                                      st(oacc_d[pi][u0:u0 + 127 * DD + 1:DD, :], ost[b][:], r=['ostF%d_%d' % (b, hh) for hh in range(8)],
                                         w=['oacc_d%d' % bi]) if h == 7 else None))
                        if len(pend) > 2:
                            pend.pop(0)()
                        if len(pendc) > 4:
                            pendc.pop(0)()
                        if h == 4 and bi + 1 < len(blocks):
                            f_load(bi + 1)
                while pend:
                    pend.pop(0)()
                while pendc:
                    pendc.pop(0)()
                S.end_phase()

        if 'G' in phases:
            with ExitStack() as pes:
                gaab = S.sb("gaab", [128, 2048], F32, pes)
                oa = [[S.sb("oaG%d_%d" % (i, p), [128, 1032], F32, pes) for p in range(3)] for i in range(2)]
                rden = [S.sb("rdenG%d" % i, [128, 8], F32, pes) for i in range(2)]
                cat = [S.sb("catG%d" % i, [128, 2048], BF16, pes) for i in range(2)]
                catT = [S.sb("catTG%d" % i, [128, 16, 128], BF16, pes) for i in range(2)]
                xg = [S.sb("xG%d" % i, [128, 2048], F32, pes) for i in range(2)]
                tmpg = S.sb("tmpG", [128, 2048], F32, pes)
                x1s = [S.sb("x1sG%d" % i, [128, 2048], F32, pes) for i in range(2)]
                ps_tr = [S.ps("ps_trG%d" % i, [128, 1024], BF16, pes) for i in range(2)]
                ps_m = [S.ps("ps_mG%d" % i, [128, 512], F32, pes) for i in range(4)]
                wo_all = ['wo%d_%d' % (kc, pc) for kc in range(16) for pc in range(2)]
                ld(gaab[:], mod_flat[4096:6144].partition_broadcast(128), w=['gaab'])
                def g_stage1a(t):
                    b = t % 2
                    for p in range(3):
                        ld(oa[b][p][:], oacc_d[p][t * 128:(t + 1) * 128, :], w=['oaG%d_%d' % (b, p)])
                    ld(cat[b][:, 0:1024], cat_d[t][:, 0:1024], w=['catG%d_g' % b])
                    ld(xg[b][:], x_loc[OWN0 + t * 128:OWN0 + (t + 1) * 128, :], w=['xG%d' % b])
                    tt('pool', oa[b][0][:], oa[b][0][:], oa[b][1][:], ALU.add, r=['oaG%d_0' % b, 'oaG%d_1' % b], w=['oaG%d_0' % b])
                    tt('pool', oa[b][0][:], oa[b][0][:], oa[b][2][:], ALU.add, r=['oaG%d_0' % b, 'oaG%d_2' % b], w=['oaG%d_0' % b])

                def g_stage1b(t):
                    b = t % 2
                    acc3 = oa[b][0][:].rearrange("p (a c) -> p a c", a=8)
                    S.op('dve', lambda e, acc3=acc3, b=b: e.reciprocal(out=rden[b][:], in_=acc3[:, :, 128]), r=['oaG%d_0' % b], w=['rdenG%d' % b])
                    for h in range(8):
                        ts('dve', cat[b][:, 1024 + h * 128:1024 + (h + 1) * 128], acc3[:, h, 0:128], rden[b][:, h:h + 1], None, ALU.mult, None,
                           r=['oaG%d_0' % b, 'rdenG%d' % b], w=['catG%d_d%d' % (b, h)])

                def g_stage2a(t):
                    b = t % 2
                    cat_all = ['catG%d_g' % b] + ['catG%d_d%d' % (b, h) for h in range(8)]
                    for kc in range(16):
                        hf, k8 = kc // 8, kc % 8
                        tr(ps_tr[hf][:, k8 * 128:(k8 + 1) * 128], cat[b][:, kc * 128:(kc + 1) * 128], ident_b[:],
                           r=cat_all + ['ident_b'], w=['ps_trG%d' % hf])
                    cp('dve', catT[b][:, 0:8, :], ps_tr[0][:, :].rearrange("p (a c) -> p a c", a=8), r=['ps_trG0'], w=['catTG%d_0' % b])
                    act(catT[b][:, 8:16, :], ps_tr[1][:, :].rearrange("p (a c) -> p a c", a=8), AF.Copy, r=['ps_trG1'], w=['catTG%d_1' % b])

                def g_stage2b(t):
                    b = t % 2
                    for n in range(4):
                        for kc in range(16):
                            mm(ps_m[n][:, :], catT[b][:, kc, :], wo[:, kc, n * 512:(n + 1) * 512], kc == 0, kc == 15,
                               r=['catTG%d_0' % b, 'catTG%d_1' % b] + wo_all, w=['ps_mG%d' % n])
                        nsl = slice(n * 512, (n + 1) * 512)
                        tt('dve', tmpg[:, nsl], ps_m[n][:, :], gaab[:, nsl], ALU.mult, r=['ps_mG%d' % n, 'gaab'], w=['tmpG%d' % n])
                        tt('dve', x1s[b][:, nsl], tmpg[:, nsl], xg[b][:, nsl], ALU.add, r=['tmpG%d' % n, 'xG%d' % b], w=['x1sG%d_%d' % (b, n)])
                    st(x1_d[t], x1s[b][:], r=['x1sG%d_%d' % (b, n) for n in range(4)], w=['x1_d%d' % t])

                g_stage1a(0)
                g_stage1b(0)
                for t in range(16):
                    if t + 1 < 16:
                        g_stage1a(t + 1)
                    g_stage2a(t)
                    if t + 1 < 16:
                        g_stage1b(t + 1)
                    g_stage2b(t)
                S.end_phase()

        pre_wo.close()
        if 'H' in phases:
            S.barrier(include_bg=True)
            with ExitStack() as pes:
                NBR = 6
                NBV = 4
                HT = int(os.environ.get("HTILES", "16"))
                wq = S.sb("wq", [128, 16, 2048], BF16, pes)
                keysb = S.sb("keysb", [128, 16, 128], BF16, pes)
                gafb = S.sb("gafb", [128, 2048], F32, pes)
                gfinb = S.sb("gfinb", [128, 2048], F32, pes)
                iota16 = S.sb("iota16", [128, 16], F32, pes)
                x1t = S.sb("x1t", [128, 2048], F32, pes)
                x1e = S.sb("x1e", [128, 2048], F32, pes)
                tmpE = S.sb("tmpE", [128, 512], F32, pes)
                xnb = S.sb("xnbH", [128, 2048], BF16, pes)
                h2T = S.sb("h2T", [128, 16, 128], BF16, pes)
                h2b = [S.sb("h2b%d" % i, [128, 2048], BF16, pes) for i in range(2)]
                qTs = S.sb("qTs", [128, 16, 128], BF16, pes)
                sc = S.sb("scH", [128, 16, 128], F32, pes)
                sc2 = [S.sb("sc2H%d" % i, [128, 128], F32, pes) for i in range(2)]
                tv = S.sb("tvH", [128, 16, 16], F32, pes)
                ti = S.sb("tiH", [128, 16, 16], U32, pes)
                tif = S.sb("tifH", [128, 16, 16], F32, pes)
                cand = [S.sb("candH%d" % i, [128, 16, 16], F32, pes) for i in range(2)]
                cand2 = [S.sb("cand2H%d" % i, [128, 256], F32, pes) for i in range(2)]
                bs = S.sb("bsH", [128, 8, 16], F32, pes)
                posu = S.sb("posuH", [128, 8, 16], U32, pes)
                au = S.sb("auH", [128, 128], U32, pes)
                bu = S.sb("buH", [128, 128], U32, pes)
                af = S.sb("afH", [128, 128], F32, pes)
                bf = S.sb("bfH", [128, 128], F32, pes)
                i0 = S.sb("i0H", [128, 128], F32, pes)
                i1 = S.sb("i1H", [128, 128], F32, pes)
                idxf = S.sb("idxfH", [128, 128], F32, pes)
                idxu = [S.sb("idxuH%d" % i, [128, 128], U32, pes) for i in range(3)]
                nm = S.sb("nmH", [128, 8], F32, pes)
                Zs = S.sb("ZsH", [128, 8], F32, pes)
                rZ = S.sb("rZH", [128, 8], F32, pes)
                ge = S.sb("geH", [128, 8, 16], F32, pes)
                gate = [S.sb("gateH%d" % i, [128, 8, 16], F32, pes) for i in range(3)]
                a_all = [S.sb("a_allH%d" % i, [128, 128], F32, pes) for i in range(2)]
                ga = S.sb("gaH", [128, 128], F32, pes)
                wgt = [S.sb("wgtH%d" % i, [128, 128], F32, pes) for i in range(2)]
                gbU = [S.sb("gbU%d" % i, [128, 2048], BF16, pes) for i in range(NBR)]
                gbV = [S.sb("gbV%d" % i, [128, 2048], BF16, pes) for i in range(NBV)]
                prod = [S.sb("prodH%d" % i, [128, 2048], BF16, pes) for i in range(2)]
                pcn = [0]
                dg = [S.sb("dgH%d" % i, [128, 128], BF16, pes) for i in range(4)]
                junkb = S.sb("junkbH", [128, 2048], BF16, pes)
                junka = S.sb("junkaH", [128, 2048], BF16, pes)
                st4 = S.sb("st4H", [128, 8], F32, pes)
                ps_pro = [S.ps("ps_proH%d" % i, [128, 512], F32, pes) for i in range(4)]
                ps_big = [S.ps("ps_bigH%d" % i, [128, 512], F32, pes) for i in range(4)]
                for kc in range(16):
                    for pc in range(2):
                        ld(wq[:, kc, pc * 1024:(pc + 1) * 1024], w_pq[kc * 128:(kc + 1) * 128, pc * 1024:(pc + 1) * 1024],
                           w=['wq%d_%d' % (kc, pc)], q='pool')
                wq_all = ['wq%d_%d' % (kc, pc) for kc in range(16) for pc in range(2)]
                for pc in range(2):
                    ld(keysb[:, pc * 8:(pc + 1) * 8, :], keysT_in[:, pc * 8:(pc + 1) * 8, :], w=['keysb%d' % pc], q='pool')
                ld(iota16[:], cst["iota16"], w=['iota16'])
                ld(gafb[:], mod_flat[10240:12288].partition_broadcast(128), w=['gafb'])
                ld(gfinb[:], gfin_row[0, :].partition_broadcast(128), w=['gfinb'])
                pa = list(tv[:].ap[0])

                def bcast_ap(tile_ap, dims):
                    return bass.AP(tile_ap.tensor, tile_ap.offset, [list(tile_ap.ap[0])] + dims)

                def pro_bf(i):
                    return ps_pro[i][:, :].bitcast(BF16)

                def prologue(t):
                    s2, s3 = t % 2, t % 3
                    ld(x1t[:], x1_d[t], r=['x1_d%d' % t], w=['x1t'])
                    act(junka[:], x1t[:], AF.Square, r=['x1t'], w=['junkaH', 'H0ss'], accum_out=st4[:, 0:1])
                    rstd_from_ss(st4[:, 0:1], st4[:, 1:2], st4[:, 2:3], st4[:, 3:4], D, 'H0')
                    act(xnb[:], x1t[:], AF.Copy, r=['x1t', 'H0rstd'], w=['xnbH'], scale=st4[:, 3:4])
                    yield
                    for kc in range(16):
                        hf, k8 = kc // 8, kc % 8
                        tr(pro_bf(hf)[:, k8 * 128:(k8 + 1) * 128], xnb[:, kc * 128:(kc + 1) * 128], ident_b[:],
                           r=['xnbH', 'ident_b'], w=['ps_proH%d' % hf])
                        if kc % 4 == 3:
                            yield
                    for kc in range(16):
                        hf, k8 = kc // 8, kc % 8
                        if hf == 0:
                            ts('dve', h2T[:, kc, :], pro_bf(hf)[:, k8 * 128:(k8 + 1) * 128], gscfT[:, kc:kc + 1], modT[:, 48 + kc:49 + kc],
                               ALU.mult, ALU.add, r=['ps_proH0', 'gscfT', 'modT'], w=['h2T_%d' % kc])
                        else:
                            act(h2T[:, kc, :], pro_bf(hf)[:, k8 * 128:(k8 + 1) * 128], AF.Identity, r=['ps_proH1', 'gscfT', 'modT'],
                                w=['h2T_%d' % kc], bias=modT[:, 48 + kc:49 + kc], scale=gscfT[:, kc:kc + 1])
                        if kc % 4 == 3:
                            yield
                    h2T_all = ['h2T_%d' % kc for kc in range(16)]
                    for kc in range(16):
                        hf, k8 = kc // 8, kc % 8
                        tr(pro_bf(2 + hf)[:, k8 * 128:(k8 + 1) * 128], h2T[:, kc, :], ident_b[:],
                           r=['h2T_%d' % kc, 'ident_b'], w=['ps_proH%d' % (2 + hf)])
                        if kc % 4 == 3:
                            yield
                    cp('dve', h2b[s2][:, 0:1024], pro_bf(2)[:, :], r=['ps_proH2'], w=['h2b%d_0' % s2])
                    act(h2b[s2][:, 1024:2048], pro_bf(3)[:, :], AF.Copy, r=['ps_proH3'], w=['h2b%d_1' % s2])
                    yield
                    for c in range(16):
                        pb = c % 2
                        for kc in range(16):
                            mm(ps_pro[pb][:, 0:128], wq[:, kc, c * 128:(c + 1) * 128], h2T[:, kc, :], kc == 0, kc == 15,
                               r=h2T_all + wq_all, w=['ps_proH%d' % pb])
                        yield
                        if pb == 0:
                            act(qTs[:, c, :], ps_pro[pb][:, 0:128], AF.Copy, r=['ps_proH0'], w=['qTs%d' % c])
                        else:
                            cp('dve', qTs[:, c, :], ps_pro[pb][:, 0:128], r=['ps_proH1'], w=['qTs%d' % c])
                        mm(ps_pro[2 + pb][:, 0:128], qTs[:, c, :], keysb[:, c, :], True, True,
                           r=['qTs%d' % c, 'keysb0', 'keysb1'], w=['ps_proH%d' % (2 + pb)])
                        if pb == 0:
                            cp('dve', sc[:, c, :], ps_pro[2][:, 0:128], r=['ps_proH2'], w=['scH%d' % c])
                        else:
                            act(sc[:, c, :], ps_pro[3][:, 0:128], AF.Copy, r=['ps_proH3'], w=['scH%d' % c])
                        yield
                    for c in range(16):
                        rb = c % 2
                        sn = 'scH%d' % c
                        S.op('dve', lambda e, c=c: e.max(out=tv[:, c, 0:8], in_=sc[:, c, :]), r=[sn], w=['tvH%d_a' % c])
                        S.op('dve', lambda e, c=c: e.max_index(out=ti[:, c, 0:8], in_max=tv[:, c, 0:8], in_values=sc[:, c, :]),
                             r=[sn, 'tvH%d_a' % c], w=['tiH%d_a' % c])
                        S.op('dve', lambda e, c=c, rb=rb: e.match_replace(out=sc2[rb][:], in_to_replace=tv[:, c, 0:8], in_values=sc[:, c, :],
                                                                       imm_value=-1e30), r=[sn, 'tvH%d_a' % c], w=['sc2H%d' % rb])
                        S.op('dve', lambda e, c=c, rb=rb: e.max(out=tv[:, c, 8:16], in_=sc2[rb][:]), r=['sc2H%d' % rb], w=['tvH%d_b' % c])
                        S.op('dve', lambda e, c=c, rb=rb: e.max_index(out=ti[:, c, 8:16], in_max=tv[:, c, 8:16], in_values=sc2[rb][:]),
                             r=['sc2H%d' % rb, 'tvH%d_b' % c], w=['tiH%d_b' % c])
                        yield
                    tv_all = ['tvH%d_%s' % (c, x) for c in range(16) for x in 'ab']
                    ti_all = ['tiH%d_%s' % (c, x) for c in range(16) for x in 'ab']
                    cp('dve', tif[:], ti[:], r=ti_all, w=['tifH'])
                    for h in range(8):
                        rb = h % 2
                        in0 = bcast_ap(tv[:, 2 * h, :], [[1, 16], [0, 16]])
                        in1 = bcast_ap(tv[:, 2 * h + 1, :], [[0, 16], [1, 16]])
                        tt('dve', cand[rb][:], in0, in1, ALU.add, r=tv_all, w=['candH%d' % rb])
                        cf = cand[rb][:].rearrange("p a b -> p (a b)")
                        S.op('dve', lambda e, h=h, cf=cf: e.max(out=bs[:, h, 0:8], in_=cf), r=['candH%d' % rb], w=['bsH%d_a' % h])
                        S.op('dve', lambda e, h=h, cf=cf: e.max_index(out=posu[:, h, 0:8], in_max=bs[:, h, 0:8], in_values=cf),
                             r=['candH%d' % rb, 'bsH%d_a' % h], w=['posuH%d_a' % h])
                        S.op('dve', lambda e, h=h, cf=cf, rb=rb: e.match_replace(out=cand2[rb][:], in_to_replace=bs[:, h, 0:8], in_values=cf,
                                                                              imm_value=-1e30), r=['candH%d' % rb, 'bsH%d_a' % h], w=['cand2H%d' % rb])
                        S.op('dve', lambda e, h=h, rb=rb: e.max(out=bs[:, h, 8:16], in_=cand2[rb][:]), r=['cand2H%d' % rb], w=['bsH%d_b' % h])
                        S.op('dve', lambda e, h=h, rb=rb: e.max_index(out=posu[:, h, 8:16], in_max=bs[:, h, 8:16], in_values=cand2[rb][:]),
                             r=['cand2H%d' % rb, 'bsH%d_b' % h], w=['posuH%d_b' % h])
                        yield
                    bs_all = ['bsH%d_%s' % (h, x) for h in range(8) for x in 'ab']
                    pos_all = ['posuH%d_%s' % (h, x) for h in range(8) for x in 'ab']
                    posf = posu[:].rearrange("p a b -> p (a b)")
                    S.op('dve', lambda e: e.tensor_single_scalar(out=au[:], in_=posf, scalar=4, op=ALU.logical_shift_right), r=pos_all, w=['auH'])
                    S.op('dve', lambda e: e.tensor_single_scalar(out=bu[:], in_=posf, scalar=15, op=ALU.bitwise_and), r=pos_all, w=['buH'])
                    cp('dve', af[:], au[:], r=['auH'], w=['afH'])
                    cp('dve', bf[:], bu[:], r=['buH'], w=['bfH'])
                    yield
                    eq3 = x1t[:].rearrange("p (a b) -> p a b", b=16)
                    eq4 = x1t[:].rearrange("p (h k b) -> p h k b", h=8, k=16)
                    io_b = bcast_ap(iota16[:], [[0, 128], [1, 16]])
                    for sidx, (xf, xname, iout, iname) in enumerate([(af, 'afH', i0, 'i0H'), (bf, 'bfH', i1, 'i1H')]):
                        xb = bcast_ap(xf[:], [[1, 128], [0, 16]])
                        tt('dve', eq3, xb, io_b, ALU.is_equal, r=[xname, 'iota16'], w=['x1t'])
                        yield
                        tib = bass.AP(tif[:].tensor, tif[:].offset + sidx * 16, [pa, [32, 8], [0, 16], [1, 16]])
                        tt('dve', eq4, eq4, tib, ALU.mult, r=['x1t', 'tifH'], w=['x1t'])
                        yield
                        S.op('dve', lambda e, iout=iout: e.tensor_reduce(out=iout[:], in_=eq3, axis=AX.X, op=ALU.add), r=['x1t'], w=[iname])
                        yield
                    stt(idxf[:], i0[:], 128.0, i1[:], ALU.mult, ALU.add, r=['i0H', 'i1H'], w=['idxfH'])
                    cp('dve', idxu[s3][:], idxf[:], r=['idxfH'], w=['idxuH%d' % s3])
                    ts('dve', nm[:], bs[:, :, 0], -1.0, None, ALU.mult, None, r=bs_all, w=['nmH'])
                    for h in range(8):
                        act(ge[:, h, :], bs[:, h, :], AF.Exp, r=bs_all + ['nmH'], w=['geH%d' % h, 'ZsH%d' % h], bias=nm[:, h:h + 1], scale=1.0,
                            accum_out=Zs[:, h:h + 1])
                    S.op('dve', lambda e: e.reciprocal(out=rZ[:], in_=Zs[:]), r=['ZsH%d' % h for h in range(8)], w=['rZH'])
                    tt('dve', gate[s3][:], ge[:], bcast_ap(rZ[:], [[1, 8], [0, 16]]), ALU.mult, r=['geH%d' % h for h in range(8)] + ['rZH'],
                       w=['gateH%d' % s3])
                    yield

                gcU = [0]
                gcV = [0]
                dcnt = [0]

                def down_step(t, k):
                    s2, s3 = t % 2, t % 3
                    rg = gcU[0] % NBR
                    gcU[0] += 1
                    S.dma('pool', lambda e, rg=rg, k=k, s3=s3: e.indirect_dma_start(
                        out=gbU[rg][:], out_offset=None, in_=ub_d,
                        in_offset=bass.IndirectOffsetOnAxis(ap=idxu[s3][:, k:k + 1], axis=0)), r=['idxuH%d' % s3], w=['gbU%d' % rg])
                    h2n = ['h2b%d_0' % s2, 'h2b%d_1' % s2]
                    if k % 3 != 0:
                        pp = pcn[0] % 2
                        pcn[0] += 1
                        tt('dve', prod[pp][:], gbU[rg][:], h2b[s2][:], ALU.mult, r=['gbU%d' % rg] + h2n, w=['prodH%d' % pp])
                        act(junka[:, 0:2048], prod[pp][:], AF.Copy, r=['prodH%d' % pp], w=['junkaH', 'a_allH%d_%d' % (s2, k)],
                            accum_out=a_all[s2][:, k:k + 1])
                    else:
                        S.op('dve', lambda e, rg=rg, k=k, s2=s2: e.scalar_tensor_tensor(
                            out=junkb[:], in0=gbU[rg][:], scalar=1.0, in1=h2b[s2][:], op0=ALU.mult, op1=ALU.mult,
                            accum_out=a_all[s2][:, k:k + 1]), r=['gbU%d' % rg] + h2n, w=['junkbH', 'a_allH%d_%d' % (s2, k)])

                def up_step(t, k):
                    s2, s3 = t % 2, t % 3
                    rg = gcV[0] % NBV
                    gcV[0] += 1
                    dd = dcnt[0] % 4
                    dcnt[0] += 1
                    S.dma('pool', lambda e, rg=rg, k=k, s3=s3: e.indirect_dma_start(
                        out=gbV[rg][:], out_offset=None, in_=vb_d,
                        in_offset=bass.IndirectOffsetOnAxis(ap=idxu[s3][:, k:k + 1], axis=0)), r=['idxuH%d' % s3], w=['gbV%d' % rg])
                    act(dg[dd][:], ident_f[:], AF.Copy, r=['ident_f', 'wgtH%d' % s2], w=['dgH%d' % dd], scale=wgt[s2][:, k:k + 1])
                    for n in range(4):
                        mm(ps_big[n][:, :], dg[dd][:], gbV[rg][:, n * 512:(n + 1) * 512], k == 0, k == 127,
                           r=['dgH%d' % dd, 'gbV%d' % rg], w=['ps_bigH%d' % n])

                def finish_down(t):
                    s2, s3 = t % 2, t % 3
                    a_names = ['a_allH%d_%d' % (s2, k) for k in range(128)]
                    act(ga[:], a_all[s2][:], AF.Gelu, r=a_names, w=['gaH'])
                    tt('dve', wgt[s2][:], ga[:], gate[s3][:].rearrange("p a b -> p (a b)"), ALU.mult, r=['gaH', 'gateH%d' % s3], w=['wgtH%d' % s2])

                def epilogue(t):
                    for n in range(4):
                        nsl = slice(n * 512, (n + 1) * 512)
                        tt('dve', tmpE[:], ps_big[n][:, :], gafb[:, nsl], ALU.mult, r=['ps_bigH%d' % n, 'gafb'], w=['tmpE'])
                        tt('dve', x1e[:, nsl], x1e[:, nsl], tmpE[:], ALU.add, r=['x1e', 'tmpE'], w=['x1e'])
                    act(junka[:], x1e[:], AF.Square, r=['x1e'], w=['junkaH', 'H1ss'], accum_out=st4[:, 4:5])
                    rstd_from_ss(st4[:, 4:5], st4[:, 5:6], st4[:, 6:7], st4[:, 7:8], D, 'H1')
                    stt(x1e[:], x1e[:], st4[:, 7:8], gfinb[:], ALU.mult, ALU.mult, r=['x1e', 'H1rstd', 'gfinb'], w=['x1e'])
                    st(out_d[t * 128:(t + 1) * 128, :], x1e[:], r=['x1e'], w=['out_d%d' % t], final=True)

                for _ in prologue(0):
                    pass
                for i in range(HT + 1):
                    gen = prologue(i + 1) if i + 1 < HT else None
                    if i >= 1:
                        ld(x1e[:], x1_d[i - 1], r=['x1_d%d' % (i - 1)], w=['x1e'])
                    for k in range(128):
                        if i < HT:
                            down_step(i, k)
                        if i >= 1:
                            up_step(i - 1, k)
                        if gen is not None:
                            try:
                                next(gen)
                            except StopIteration:
                                gen = None
                    if gen is not None:
                        for _ in gen:
                            pass
                    if i < HT:
                        finish_down(i)
                    if i >= 1:
                        epilogue(i - 1)
                S.end_phase()

        S.finish()
    return nc


def prep_inputs(inputs):
    g = {k: np.asarray(v) for k, v in inputs.items()}
    l = 0
    consts = _consts()
    w_in = np.ascontiguousarray(g["w_in"][l])
    keysT = np.ascontiguousarray(g["peer_sub_keys"][l].reshape(16, 128, 128).transpose(2, 0, 1))
    shared = {
        "w_ada": np.ascontiguousarray(g["w_ada"][l]),
        "b_ada_row": np.ascontiguousarray(g["b_ada"][l].reshape(1, 6 * D)),
        "g_mixT": np.ascontiguousarray(g["g_norm_mix"][l].reshape(16, 128).T),
        "g_ffnT": np.ascontiguousarray(g["g_norm_ffn"][l].reshape(16, 128).T),
        "g_ffn_row": np.ascontiguousarray(g["g_norm_ffn"][l].reshape(1, D)),
        "g_fin_row": np.ascontiguousarray(g["g_final"].reshape(1, D)),
        "g_gla_row": np.ascontiguousarray(g["g_gla_out"][l].reshape(1, 1024)),
        "w_in": w_in,
        "w_out": np.ascontiguousarray(g["w_out"][l]),
        "w_pq": np.ascontiguousarray(g["w_peer_q"][l]),
        "keysT": keysT,
        "peer_u": np.ascontiguousarray(g["peer_u"][l]),
        "peer_v": np.ascontiguousarray(g["peer_v"][l]),
    }
    for k, v in consts.items():
        shared["c_" + k] = v
    zeros = np.zeros((16, 512), np.float32)
    wgf, wgb = g["w_gate_f"][l], g["w_gate_b"][l]
    bgf, bgb = g["b_gate_f"][l], g["b_gate_b"][l]
    gzf_cols = w_in[:, 3072:3088]
    gzb_cols = w_in[:, 3088:3104]
    in_maps = []
    for b in range(4):
        for j in range(2):
            m = dict(shared)
            xb = g["x"][b]
            pb = g["positions"][b].astype(np.int32)
            if j == 0:
                xb = xb[::-1]
                pb = pb[::-1]
                f_w, f_b, f_cols = wgb, bgb, gzb_cols
                b_w, b_b, b_cols = wgf, bgf, gzf_cols
            else:
                f_w, f_b, f_cols = wgf, bgf, gzf_cols
                b_w, b_b, b_cols = wgb, bgb, gzb_cols
            m["x_loc"] = np.ascontiguousarray(xb)
            m["pos"] = np.ascontiguousarray(pb.reshape(1, NTOK))
            m["cT"] = np.ascontiguousarray(g["c"][b].reshape(16, 128).T)
            m["w_gz"] = np.ascontiguousarray(np.concatenate([f_cols, b_cols], axis=1))
            m["Wg"] = np.ascontiguousarray(np.concatenate([
                np.concatenate([f_w, zeros], axis=1),
                np.concatenate([zeros, b_w], axis=1),
                np.concatenate([f_b, b_b])[None, :]], axis=0).astype(np.float32))
            in_maps.append(m)
    return in_maps


def kernel(**inputs):
    in_maps = prep_inputs(inputs)
    nc = build()
    res = run_bass_kernel_spmd(nc, in_maps, core_ids=list(range(8)))
    out = np.empty((4, 4096, D), np.float32)
    for b in range(4):
        for j in range(2):
            o = np.asarray(res.results[b * 2 + j]["out"])
            if j == 0:
                out[b, :2048] = o[::-1]
            else:
                out[b, 2048:] = o
    return out
```

```python
import os
import numpy as np
from contextlib import ExitStack
import concourse.bass as bass
import concourse.mybir as mybir
from concourse.bass_utils import run_bass_kernel_spmd

F32 = mybir.dt.float32
BF16 = mybir.dt.bfloat16
U32 = mybir.dt.uint32
I32 = mybir.dt.int32
ALU = mybir.AluOpType
AF = mybir.ActivationFunctionType
AX = mybir.AxisListType

ENGS = ['pe', 'act', 'dve', 'pool', 'sp']


class Sched:
    def __init__(self, nc, es, n_dma_sems=24):
        self.nc = nc
        self.es = es
        self.prog = {e: [] for e in ENGS}
        self.cnt = {e: 0 for e in ENGS}
        self.sems = {}
        for e in ENGS:
            self.sems[e] = es.enter_context(nc.semaphore("sem_" + e))
        self.dq = {}
        for q, qe in [('sp', 'sp'), ('pool', 'pool'), ('act', 'act'), ('bg', 'pool')]:
            pool = []
            for i in range(n_dma_sems):
                k = "dq_%s_%d" % (q, i)
                self.sems[k] = es.enter_context(nc.semaphore(k))
                pool.append(k)
            self.dq[q] = {'pool': pool, 'uses': [0] * n_dma_sems, 'next': 0, 'eng': qe}
        self.waited = {}
        self.W = {}
        self.R = {}
        self.finals = []
        self.nalloc = 0

    def sb(self, name, shape, dt, es=None):
        return (es or self.es).enter_context(self.nc.sbuf_tensor(name, list(shape), dt))

    def ps(self, name, shape, dt, es=None):
        return (es or self.es).enter_context(self.nc.psum_tensor(name, list(shape), dt))

    def _wait(self, eng, tok):
        sk, val = tok
        if self.waited.get((eng, sk), 0) >= val:
            return
        self.waited[(eng, sk)] = val
        sem = self.sems[sk]
        self.prog[eng].append(lambda e, sem=sem, val=val: e.wait_ge(sem, val))

    def _deps(self, eng, r, w):
        for n in r:
            for tok in self.W.get(n, ()):
                if tok[0] == eng and eng == 'pe':
                    continue
                self._wait(eng, tok)
            if n.startswith('ps'):
                for tok in self.R.get(n, ()):
                    if tok[0] == eng:
                        continue
                    self._wait(eng, tok)
        for n in w:
            for tok in self.W.get(n, ()):
                if tok[0] == eng and eng == 'pe':
                    continue
                self._wait(eng, tok)
            for tok in self.R.get(n, ()):
                if tok[0] == eng and eng == 'pe':
                    continue
                self._wait(eng, tok)

    def _commit(self, tok, r, w):
        for n in r:
            lst = self.R.setdefault(n, [])
            lst[:] = [t for t in lst if t[0] != tok[0]]
            lst.append(tok)
        for n in w:
            self.W[n] = [tok]
            self.R[n] = []

    def op(self, eng, fn, r=(), w=()):
        self._deps(eng, r, w)
        self.cnt[eng] += 1
        tok = (eng, self.cnt[eng])
        sem = self.sems[eng]
        self.prog[eng].append(lambda e, fn=fn, sem=sem: fn(e).then_inc(sem, 1))
        self._commit(tok, r, w)
        return tok

    def dma(self, qn, fn, r=(), w=(), final=False):
        d = self.dq[qn]
        q = d['eng']
        self._deps(q, r, w)
        i = d['next']
        d['next'] = (i + 1) % len(d['pool'])
        sk = d['pool'][i]
        if d['uses'][i] > 0:
            self._wait(q, (sk, 16 * d['uses'][i]))
        d['uses'][i] += 1
        tok = (sk, 16 * d['uses'][i])
        sem = self.sems[sk]
        self.prog[q].append(lambda e, fn=fn, sem=sem: fn(e).then_inc(sem, 16))
        self._commit(tok, r, w)
        if final:
            self.finals.append(tok)
        return tok

    def barrier(self, include_bg=False):
        toks = [(e, self.cnt[e]) for e in ENGS if self.cnt[e] > 0]
        for q, d in self.dq.items():
            if q == 'bg' and not include_bg:
                continue
            for i, sk in enumerate(d['pool']):
                if d['uses'][i] > 0:
                    toks.append((sk, 16 * d['uses'][i]))
        for e in ENGS:
            for tok in toks:
                if tok[0] == e:
                    continue
                self._wait(e, tok)
        keepW = {k: v for k, v in self.W.items() if k.startswith('bg_')}
        self.W.clear()
        self.R.clear()
        self.W.update(keepW)

    def flush(self):
        nc = self.nc
        prog = self.prog
        self.prog = {e: [] for e in ENGS}
        with nc.Block() as block:
            @block.tensor
            def _(e):
                for f in prog['pe']:
                    f(e)

            @block.scalar
            def _(e):
                for f in prog['act']:
                    f(e)

            @block.vector
            def _(e):
                for f in prog['dve']:
                    f(e)

            @block.gpsimd
            def _(e):
                for f in prog['pool']:
                    f(e)

            @block.sync
            def _(e):
                for f in prog['sp']:
                    f(e)

    def end_phase(self):
        self.barrier()
        self.flush()

    def finish(self):
        for tok in self.finals:
            self._wait('sp', tok)
        self.flush()


D = 2048
NTOK = 4096
OWN0 = 2048
EPS = 1e-6
TWO_PI = 6.283185307179586
PI = 3.141592653589793


def _consts():
    j = np.arange(128)[:, None]
    i = np.arange(128)[None, :]
    c = {}
    c["ident_f"] = np.eye(128, dtype=np.float32)
    c["LT_f"] = (j <= i).astype(np.float32)
    c["LT_b"] = (j >= i).astype(np.float32)
    c["pswap"] = (j == (i + 64) % 128).astype(np.float32)
    half = 64
    inv = np.power(10000.0, -np.arange(half, dtype=np.float32) * 2.0 / 128.0).astype(np.float32)
    c["invf"] = np.concatenate([inv, inv]).reshape(128, 1).astype(np.float32)
    sgn = np.concatenate([-np.ones(64), np.ones(64)]).reshape(128, 1).astype(np.float32)
    c["sgn"] = sgn
    mA = (j >= i).astype(np.float32)
    mB = (j <= i).astype(np.float32)
    mBl = mB * (j < 64)
    c["maskAB"] = np.concatenate([mA, mB], axis=1).astype(np.float32)
    c["maskABl"] = np.concatenate([mA, mBl], axis=1).astype(np.float32)
    c["ones_col"] = np.ones((128, 1), np.float32)
    c["iota16"] = np.tile(np.arange(16, dtype=np.float32)[None, :], (128, 1))
    return c


CONST_SHAPES = {k: v.shape for k, v in _consts().items()}


def build(phases="ABCDEFGH", dbg=()):
    nc = bass.Bass("TRN2", target_bir_lowering=False)

    def din(name, shape, dt=F32):
        return nc.dram_tensor(name, list(shape), dt, kind="ExternalInput").ap()

    def dscr(name, shape, dt):
        kind = "ExternalOutput" if name in dbg else "Internal"
        return nc.dram_tensor(name, list(shape), dt, kind=kind).ap()

    x_loc = din("x_loc", [NTOK, D])
    pos_in = din("pos", [1, NTOK], I32)
    cT_in = din("cT", [128, 16])
    w_ada = din("w_ada", [D, 6 * D])
    badaT_in = din("b_ada_row", [1, 6 * D])
    gmixT_in = din("g_mixT", [128, 16])
    gffnT_in = din("g_ffnT", [128, 16])
    gffn_row = din("g_ffn_row", [1, D])
    gfin_row = din("g_fin_row", [1, D])
    ggla_row = din("g_gla_row", [1, 1024])
    w_in = din("w_in", [D, 6176])
    w_gz = din("w_gz", [D, 32])
    Wg_in = din("Wg", [33, 1024])
    w_out = din("w_out", [D, D])
    w_pq = din("w_pq", [D, D])
    keysT_in = din("keysT", [128, 16, 128])
    peer_u = din("peer_u", [16384, D])
    peer_v = din("peer_v", [16384, D])
    cst = {k: din("c_" + k, list(s)) for k, s in CONST_SHAPES.items()}
    out_d = nc.dram_tensor("out", [2048, D], F32, kind="ExternalOutput").ap()

    mod_d = dscr("mod_d", [96, 128], F32)
    hT_d = dscr("hT_d", [32, 128, 16, 128], BF16)
    gtok_d = dscr("gtok_d", [32, 128, 3072], BF16)
    gT_d = dscr("gT_d", [16, 128, 16, 128], BF16)
    el_d = dscr("el_d", [32, 128, 8], F32)
    ob_d = dscr("ob_d", [16, 128, 1024], F32)
    cat_d = dscr("cat_d", [16, 128, 2048], BF16)
    qT_d = dscr("qT_d", [8, 128, 2048], BF16)
    kT_d = dscr("kT_d", [8, 128, 4096], BF16)
    v_d = dscr("v_d", [4096, 1032], BF16)
    oacc_d = dscr("oacc_d", [3, 2048, 1032], F32)
    x1_d = dscr("x1_d", [16, 128, D], F32)
    ub_d = dscr("ub_d", [16384, D], BF16)
    vb_d = dscr("vb_d", [16384, D], BF16)

    with ExitStack() as es:
        S = Sched(nc, es)

        def mm(out, lhsT, rhs, start, stop, r, w):
            S.op('pe', lambda e: e.matmul(out, lhsT, rhs, start=start, stop=stop), r=r, w=w)

        def tr(out, in_, ident, r, w):
            S.op('pe', lambda e: e.transpose(out, in_, ident), r=r, w=w)

        def act(out, in_, func, r, w, bias=None, scale=None, accum_out=None):
            kw = {}
            if bias is not None:
                kw['bias'] = bias
            if scale is not None:
                kw['scale'] = scale
            if accum_out is not None:
                kw['accum_out'] = accum_out
            S.op('act', lambda e: e.activation(out=out, in_=in_, func=func, **kw), r=r, w=w)

        def ts(eng, out, in0, s1, s2, op0, op1, r, w):
            if op1 is None:
                S.op(eng, lambda e: e.tensor_scalar(out=out, in0=in0, scalar1=s1, scalar2=None, op0=op0), r=r, w=w)
            else:
                S.op(eng, lambda e: e.tensor_scalar(out=out, in0=in0, scalar1=s1, scalar2=s2, op0=op0, op1=op1), r=r, w=w)

        def stt(out, in0, scalar, in1, op0, op1, r, w):
            S.op('dve', lambda e: e.scalar_tensor_tensor(out=out, in0=in0, scalar=scalar, in1=in1, op0=op0, op1=op1), r=r, w=w)

        def tt(eng, out, in0, in1, op, r, w):
            S.op(eng, lambda e: e.tensor_tensor(out=out, in0=in0, in1=in1, op=op), r=r, w=w)

        def cp(eng, out, in_, r, w):
            S.op(eng, lambda e: e.tensor_copy(out=out, in_=in_), r=r, w=w)

        def ld(out, in_, w, r=(), q='sp'):
            S.dma(q, lambda e: e.dma_start(out=out, in_=in_), r=r, w=w)

        def st(out, in_, r, w, q='sp', final=False):
            S.dma(q, lambda e: e.dma_start(out=out, in_=in_), r=r, w=w, final=final)

        def rstd_from_ss(ss, var, sd, rstd, n, tag):
            ts('dve', var, ss, 1.0 / n, EPS, ALU.mult, ALU.add, r=[tag + 'ss'], w=[tag + 'var'])
            act(sd, var, AF.Sqrt, r=[tag + 'var'], w=[tag + 'sd'])
            S.op('dve', lambda e: e.reciprocal(out=rstd, in_=sd), r=[tag + 'sd'], w=[tag + 'rstd'])

        ident_f = S.sb("ident_f", [128, 128], F32)
        ident_b = S.sb("ident_b", [128, 128], BF16)
        ld(ident_f[:], cst["ident_f"], w=['ident_f'])
        cp('dve', ident_b[:], ident_f[:], r=['ident_f'], w=['ident_b'])
        modT = S.sb("modT", [128, 96], F32)
        gscaT = S.sb("gscaT", [128, 16], F32)
        gscfT = S.sb("gscfT", [128, 16], F32)
        mod_flat = mod_d.rearrange("a b -> (a b)")

        bg_list = []
        if 'H' in phases:
            for i in range(64):
                bg_list.append((ub_d[i * 256:(i + 1) * 256, :], peer_u[i * 256:(i + 1) * 256, :], 'bg_ub%d' % i))
            for i in range(64):
                bg_list.append((vb_d[i * 256:(i + 1) * 256, :], peer_v[i * 256:(i + 1) * 256, :], 'bg_vb%d' % i))

        def bg_issue(n, dep=None):
            for _ in range(n):
                if not bg_list:
                    return
                o_, i_, nm_ = bg_list.pop(0)
                S.dma('bg', lambda e, o_=o_, i_=i_: e.dma_start(out=o_, in_=i_), r=([dep] if dep else []), w=[nm_])

        if 'A' in phases:
            with ExitStack() as pes:
                c_sb = S.sb("c_sb", [128, 16], F32, pes)
                sc_sb = S.sb("sc_sb", [128, 16], F32, pes)
                gmix_sb = S.sb("gmix_sb", [128, 16], F32, pes)
                gffn_sb = S.sb("gffn_sb", [128, 16], F32, pes)
                modrow = S.sb("modrow", [1, 6 * D], F32, pes)
                badar = S.sb("badar", [1, 6 * D], F32, pes)
                one1 = S.sb("one1", [1, 1], F32, pes)
                wa = [S.sb("wa%d" % i, [128, 16, 512], F32, pes) for i in range(2)]
                ps_row = [S.ps("ps_rowA%d" % i, [128, 512], F32, pes) for i in range(2)]
                ps_mod = S.ps("ps_mod", [128, 512], F32, pes)
                ld(c_sb[:], cT_in, w=['c_sb'])
                ld(badar[:], badaT_in, w=['badar'])
                ld(gmix_sb[:], gmixT_in, w=['gmix'])
                ld(gffn_sb[:], gffnT_in, w=['gffn'])
                S.op('dve', lambda e: e.memset(one1[:], 1.0), r=[], w=['one1'])
                act(sc_sb[:], c_sb[:], AF.Silu, r=['c_sb'], w=['sc_sb'])
                w_ada_v = w_ada.rearrange("(kc p) n -> p kc n", p=128)
                for ng in range(24):
                    b = ng % 2
                    for q4 in range(4):
                        ld(wa[b][:, q4 * 4:(q4 + 1) * 4, :], w_ada_v[:, q4 * 4:(q4 + 1) * 4, ng * 512:(ng + 1) * 512],
                           w=['wa%d_%d' % (b, q4)])
                    for kc in range(16):
                        mm(ps_row[b][0:1, :], sc_sb[:, kc:kc + 1], wa[b][:, kc, :], kc == 0, kc == 15,
                           r=['wa%d_%d' % (b, kc // 4), 'sc_sb'], w=['ps_rowA%d' % b])
                    tt('dve', modrow[0:1, ng * 512:(ng + 1) * 512], ps_row[b][0:1, :], badar[0:1, ng * 512:(ng + 1) * 512], ALU.add,
                       r=['ps_rowA%d' % b, 'badar'], w=['modrow%d' % ng])
                mr_all = ['modrow%d' % ng for ng in range(24)]
                st(mod_d.rearrange("a b -> (a b)").rearrange("(o n) -> o n", o=1), modrow[:], r=mr_all, w=['mod_d'])
                for c in range(96):
                    mm(ps_mod[:, c:c + 1], modrow[0:1, c * 128:(c + 1) * 128], one1[0:1, 0:1], True, True,
                       r=mr_all + ['one1'], w=['ps_mod'])
                cp('dve', modT[:], ps_mod[:, 0:96], r=['ps_mod'], w=['modT'])
                stt(gscaT[:], modT[:, 16:32], 1.0, gmix_sb[:], ALU.add, ALU.mult, r=['modT', 'gmix'], w=['gscaT'])
                stt(gscfT[:], modT[:, 64:80], 1.0, gffn_sb[:], ALU.add, ALU.mult, r=['modT', 'gffn'], w=['gscfT'])
                S.end_phase()

        ones_col = S.sb("ones_col", [128, 1], F32)
        ld(ones_col[:], cst["ones_col"], w=['ones_col'])
        pre_wg = ExitStack()
        wg = S.sb("wg", [128, 16, 3104], BF16, pre_wg)
        if 'C' in phases:
            for kc in range(16):
                for pc in range(3):
                    ld(wg[:, kc, pc * 1024:(pc + 1) * 1024], w_in[kc * 128:(kc + 1) * 128, pc * 1024:(pc + 1) * 1024],
                       w=['wg%d_%d' % (kc, pc)], q='pool')
                ld(wg[:, kc, 3072:3104], w_gz[kc * 128:(kc + 1) * 128, :], w=['wg%d_3' % kc], q='pool')
        if 'B' in phases:
            with ExitStack() as pes:
                xt = [S.sb("xt%d" % i, [128, D], F32, pes) for i in range(2)]
                xn = [S.sb("xn%d" % i, [128, D], BF16, pes) for i in range(2)]
                hs = [S.sb("hs%d" % i, [128, 16, 128], BF16, pes) for i in range(2)]
                junk = S.sb("junkB", [128, D], BF16, pes)
                st4 = [S.sb("st4_%d" % i, [128, 4], F32, pes) for i in range(2)]
                ps_t = [[S.ps("ps_tB%d_%d" % (i, hf), [128, 1024], BF16, pes) for hf in range(2)] for i in range(2)]
                def b_stage1(t):
                    b = t % 2
                    B = str(b)
                    ld(xt[b][:], x_loc[t * 128:(t + 1) * 128, :], w=['xt' + B])
                    act(junk[:], xt[b][:], AF.Square, r=['xt' + B], w=['junkB', 'B%sss' % B], accum_out=st4[b][:, 0:1])
                    rstd_from_ss(st4[b][:, 0:1], st4[b][:, 1:2], st4[b][:, 2:3], st4[b][:, 3:4], D, 'B' + B)
                    act(xn[b][:], xt[b][:], AF.Copy, r=['xt' + B, 'B%srstd' % B], w=['xn' + B], scale=st4[b][:, 3:4])

                def b_stage2(t):
                    b = t % 2
                    B = str(b)
                    for kc in range(16):
                        hf, k8 = kc // 8, kc % 8
                        tr(ps_t[b][hf][:, k8 * 128:(k8 + 1) * 128], xn[b][:, kc * 128:(kc + 1) * 128], ident_b[:],
                           r=['xn' + B, 'ident_b'], w=['ps_tB%s_%d' % (B, hf)])
                    for kc in range(16):
                        hf, k8 = kc // 8, kc % 8
                        if hf == 0:
                            ts('dve', hs[b][:, kc, :], ps_t[b][hf][:, k8 * 128:(k8 + 1) * 128], gscaT[:, kc:kc + 1],
                               modT[:, kc:kc + 1], ALU.mult, ALU.add,
                               r=['ps_tB%s_%d' % (B, hf), 'gscaT', 'modT'], w=['hs%s_%d' % (B, kc)])
                        else:
                            act(hs[b][:, kc, :], ps_t[b][hf][:, k8 * 128:(k8 + 1) * 128], AF.Identity,
                                r=['ps_tB%s_%d' % (B, hf), 'gscaT', 'modT'], w=['hs%s_%d' % (B, kc)],
                                bias=modT[:, kc:kc + 1], scale=gscaT[:, kc:kc + 1])
                    st(hT_d[t], hs[b][:], r=['hs%s_%d' % (B, kc) for kc in range(16)], w=['hT_d%d' % t])

                b_stage1(0)
                for t in range(32):
                    if t + 1 < 32:
                        b_stage1(t + 1)
                    b_stage2(t)
                S.end_phase()

        if 'C' in phases:
            with ExitStack() as pes:
                Wg_sb = S.sb("Wg_sb", [33, 1024], F32, pes)
                LTf = S.sb("LTf", [128, 128], F32, pes)
                LTb = S.sb("LTb", [128, 128], F32, pes)
                gz_sb = S.sb("gz_sb", [33, 128], F32, pes)
                hTt = [S.sb("hTt%d" % i, [128, 16, 128], BF16, pes) for i in range(2)]
                e1 = S.sb("e1", [128, 1024], F32, pes)
                l1 = S.sb("l1", [128, 1024], F32, pes)
                epos = S.sb("epos", [128, 1024], F32, pes)
                eneg = S.sb("eneg", [128, 1024], F32, pes)
                elx = [S.sb("elx%d" % i, [128, 8], F32, pes) for i in range(2)]
                qd = S.sb("qd", [128, 1024], BF16, pes)
                tk = [S.sb("tk%d" % i, [128, 3072], BF16, pes) for i in range(2)]
                tT = [S.sb("tT%d" % i, [128, 16, 128], BF16, pes) for i in range(2)]
                ps_p = [S.ps("ps_pC%d" % i, [128, 512], F32, pes) for i in range(3)]
                ps_gz = S.ps("ps_gzC", [128, 512], F32, pes)
                ps_z = [S.ps("ps_zC%d" % i, [128, 512], F32, pes) for i in range(2)]
                ps_tr = [S.ps("ps_trC%d" % i, [128, 1024], BF16, pes) for i in range(2)]
                ld(Wg_sb[:], Wg_in, w=['Wg_sb'])
                ld(LTf[:], cst["LT_f"], w=['LTf'])
                ld(LTb[:], cst["LT_b"], w=['LTb'])
                S.op('dve', lambda e: e.memset(gz_sb[:], 1.0), r=[], w=['gz_sb'])
                wg_all = ['wg%d_%d' % (kc, pc) for kc in range(16) for pc in range(4)]
                pcnt = [0]

                def proj_chunk(b, chunk):
                    pb = pcnt[0] % 3
                    pcnt[0] += 1
                    for kc in range(16):
                        mm(ps_p[pb][:, :], hTt[b][:, kc, :], wg[:, kc, chunk * 512:(chunk + 1) * 512], kc == 0, kc == 15,
                           r=['hTt%d' % b] + wg_all, w=['ps_pC%d' % pb])
                    return pb

                ld(hTt[0][:], hT_d[0], r=['hT_d0'], w=['hTt0'])
                for t in range(32):
                    own = t >= 16
                    b = t % 2
                    if t + 1 < 32:
                        ld(hTt[1 - b][:], hT_d[t + 1], r=['hT_d%d' % (t + 1)], w=['hTt%d' % (1 - b)])
                    for kc in range(16):
                        mm(ps_gz[0:32, 0:128], wg[:, kc, 3072:3104], hTt[b][:, kc, :], kc == 0, kc == 15,
                           r=['hTt%d' % b] + wg_all, w=['ps_gzC'])
                    act(gz_sb[0:32, :], ps_gz[0:32, 0:128], AF.Copy, r=['ps_gzC'], w=['gz_sb'])
                    pv0 = proj_chunk(b, 2)
                    for hf in range(2):
                        mm(ps_z[hf][:, :], gz_sb[:, :], Wg_sb[:, hf * 512:(hf + 1) * 512], True, True,
                           r=['gz_sb', 'Wg_sb'], w=['ps_zC%d' % hf])
                    for hf in range(2):
                        act(e1[:, hf * 512:(hf + 1) * 512], ps_z[hf][:, :], AF.Exp, r=['ps_zC%d' % hf], w=['e1_%d' % hf], scale=-1.0)
                    act(l1[:], e1[:], AF.Ln, r=['e1_0', 'e1_1', 'ones_col'], w=['l1'], bias=ones_col[:, 0:1], scale=1.0)
                    act(tk[b][:, 1024:1536], ps_p[pv0][:, :], AF.Copy, r=['ps_pC%d' % pv0], w=['tk%d_v0' % b])
                    pv1 = proj_chunk(b, 3)
                    act(tk[b][:, 1536:2048], ps_p[pv1][:, :], AF.Copy, r=['ps_pC%d' % pv1], w=['tk%d_v1' % b])
                    mm(ps_z[0][:, :], LTf[:], l1[:, 0:512], True, True, r=['LTf', 'l1'], w=['ps_zC0'])
                    mm(ps_z[1][:, :], LTb[:], l1[:, 512:1024], True, True, r=['LTb', 'l1'], w=['ps_zC1'])
                    ps_elv = ps_tr[0][:, :].bitcast(F32)
                    for c in range(8):
                        mm(ps_elv[:, c:c + 1], l1[:, c * 128:(c + 1) * 128], ones_col[:, 0:1], True, True,
                           r=['l1', 'ones_col'], w=['ps_trC0'])
                    pk = proj_chunk(b, 1)
                    for hf in range(2):
                        if own or hf == 0:
                            act(eneg[:, hf * 512:(hf + 1) * 512], ps_z[hf][:, :], AF.Exp, r=['ps_zC%d' % hf], w=['eneg%d' % hf], scale=1.0 / 16)
                        if own:
                            act(epos[:, hf * 512:(hf + 1) * 512], ps_z[hf][:, :], AF.Exp, r=['ps_zC%d' % hf], w=['epos%d' % hf], scale=-1.0 / 16)
                    act(elx[b][:], ps_elv[:, 0:8], AF.Exp, r=['ps_trC0'], w=['elx%d' % b], scale=-1.0 / 16)
                    st(el_d[t], elx[b][:], r=['elx%d' % b], w=['el_d%d' % t])
                    tt('dve', tk[b][:, 0:512], ps_p[pk][:, :], eneg[:, 0:512], ALU.mult, r=['ps_pC%d' % pk, 'eneg0'], w=['tk%d_kf' % b])
                    bg_issue(2, 'tk%d_kf' % b)
                    if own:
                        tt('dve', tk[b][:, 512:1024], ps_p[pk][:, :], eneg[:, 512:1024], ALU.mult, r=['ps_pC%d' % pk, 'eneg1'], w=['tk%d_kb' % b])
                        pq = proj_chunk(b, 0)
                        stt(qd[:, 0:512], ps_p[pq][:, :], 128.0 ** -0.5, epos[:, 0:512], ALU.mult, ALU.mult,
                            r=['ps_pC%d' % pq, 'epos0'], w=['qd0'])
                        stt(qd[:, 512:1024], ps_p[pq][:, :], 128.0 ** -0.5, epos[:, 512:1024], ALU.mult, ALU.mult,
                            r=['ps_pC%d' % pq, 'epos1'], w=['qd1'])
                        for c in range(2):
                            pr = proj_chunk(b, 4 + c)
                            act(tk[b][:, 2048 + c * 512:2048 + (c + 1) * 512], ps_p[pr][:, :], AF.Silu, r=['ps_pC%d' % pr], w=['tk%d_r%d' % (b, c)])
                        for dr in range(2):
                            for kind in range(2):
                                for h in range(4):
                                    k8 = kind * 4 + h
                                    if kind == 0:
                                        srcap = qd[:, dr * 512 + h * 128: dr * 512 + (h + 1) * 128]
                                        rn = 'qd%d' % dr
                                    else:
                                        srcap = tk[b][:, dr * 512 + h * 128: dr * 512 + (h + 1) * 128]
                                        rn = 'tk%d_k%s' % (b, 'fb'[dr])
                                    tr(ps_tr[dr][:, k8 * 128:(k8 + 1) * 128], srcap, ident_b[:], r=[rn, 'ident_b'], w=['ps_trC%d' % dr])
                        cp('dve', tT[b][:, 0:8, :], ps_tr[0][:, :].rearrange("p (a b) -> p a b", a=8), r=['ps_trC0'], w=['tT%d_0' % b])
                        act(tT[b][:, 8:16, :], ps_tr[1][:, :].rearrange("p (a b) -> p a b", a=8), AF.Copy, r=['ps_trC1'], w=['tT%d_1' % b])
                        st(gT_d[t - 16], tT[b][:], r=['tT%d_0' % b, 'tT%d_1' % b], w=['gT_d%d' % (t - 16)])
                        st(gtok_d[t], tk[b][:], r=['tk%d_kf' % b, 'tk%d_kb' % b, 'tk%d_v0' % b, 'tk%d_v1' % b, 'tk%d_r0' % b, 'tk%d_r1' % b],
                           w=['gtok_d%d' % t])
                    else:
                        st(gtok_d[t][:, 0:512], tk[b][:, 0:512], r=['tk%d_kf' % b], w=['gtok_d%d_a' % t])
                        st(gtok_d[t][:, 1024:2048], tk[b][:, 1024:2048], r=['tk%d_v0' % b, 'tk%d_v1' % b], w=['gtok_d%d_b' % t])
                S.end_phase()

        pre_wg.close()
        pre_wd = ExitStack()
        wd = S.sb("wd", [128, 16, 3072], BF16, pre_wd)
        if 'E' in phases:
            for kc in range(16):
                for pc in range(3):
                    ld(wd[:, kc, pc * 1024:(pc + 1) * 1024], w_in[kc * 128:(kc + 1) * 128, 3104 + pc * 1024:3104 + (pc + 1) * 1024],
                       w=['wd%d_%d' % (kc, pc)], q='pool')
        if 'D' in phases:
            with ExitStack() as pes:
                Sf = S.sb("Sf", [128, 4, 256], F32, pes)
                Sb = S.sb("Sb", [128, 4, 256], BF16, pes)
                t1 = [S.sb("t1D%d" % i, [128, 256], F32, pes) for i in range(2)]
                mask = [S.sb("maskD%d" % i, [128, 128], F32, pes) for i in range(2)]
                gglab = S.sb("gglab", [128, 1024], F32, pes)
                tkd = [S.sb("tkd%d" % i, [128, 3072], BF16, pes) for i in range(2)]
                tTd = [S.sb("tTd%d" % i, [128, 8, 128], BF16, pes) for i in range(2)]
                eld = [S.sb("eld%d" % i, [128, 8], F32, pes) for i in range(2)]
                am = [S.sb("amD%d" % i, [128, 128], BF16, pes) for i in range(2)]
                obs = [S.sb("obs%d" % i, [128, 1024], F32, pes) for i in range(2)]
                osum = S.sb("osumD", [128, 1024], F32, pes)
                og = S.sb("ogD", [128, 1024], F32, pes)
                catg = [S.sb("catg%d" % i, [128, 1024], BF16, pes) for i in range(2)]
                junk = S.sb("junkD", [128, 256], F32, pes)
                st8 = S.sb("st8D", [128, 16], F32, pes)
                ps_at = [S.ps("ps_atD%d" % i, [128, 512], F32, pes) for i in range(2)]
                ps_o = [[S.ps("ps_oD%d_%d" % (i, hf), [128, 512], F32, pes) for hf in range(2)] for i in range(2)]
                ps_s = [S.ps("ps_sD%d" % i, [128, 512], F32, pes) for i in range(2)]
                ld(mask[0][:], cst["LT_f"], w=['maskD0'])
                ld(mask[1][:], cst["LT_b"], w=['maskD1'])
                ld(gglab[:], ggla_row[0, :].partition_broadcast(128), w=['gglab'])
                cnt = [0]

                lcnt = [0]

                def gla_load(t, dr, full):
                    i = lcnt[0]
                    lcnt[0] += 1
                    b = i % 2
                    kcol = dr * 512
                    ld(tkd[b][:, kcol:kcol + 512], gtok_d[t][:, kcol:kcol + 512], w=['tkd%d_k' % b])
                    ld(tkd[b][:, 1024:2048], gtok_d[t][:, 1024:2048], w=['tkd%d_v' % b])
                    ld(eld[b][:], el_d[t], w=['eld%d' % b])
                    if full:
                        ld(tTd[b][:], gT_d[t - 16][:, dr * 8:(dr + 1) * 8, :], w=['tTd%d' % b])
                        if dr == 0:
                            ld(tkd[b][:, 2048:3072], gtok_d[t][:, 2048:3072], w=['tkd%d_r' % b])
                            ld(obs[b][:], ob_d[t - 16], r=['ob_d%d' % (t - 16)], w=['obs%d' % b])

                def gla_tile(t, dr, full):
                    i = cnt[0]
                    cnt[0] += 1
                    b = i % 2
                    kcol = dr * 512
                    def head_at(h):
                        pa = (i * 4 + h) % 2
                        mm(ps_at[pa][:, 0:128], tTd[b][:, 4 + h, :], tTd[b][:, h, :], True, True,
                           r=['tTd%d' % b], w=['ps_atD%d' % pa])
                        return pa

                    def head_mid(h, pa):
                        vh = tkd[b][:, 1024 + h * 256:1024 + (h + 1) * 256]
                        hf = h // 2
                        ocol = (h % 2) * 256
                        if full:
                            tt('dve', am[pa][:], ps_at[pa][:, 0:128], mask[dr][:], ALU.mult, r=['ps_atD%d' % pa, 'maskD%d' % dr], w=['amD%d' % pa])
                            mm(ps_o[b][hf][:, ocol:ocol + 256], am[pa][:], vh, True, False,
                               r=['amD%d' % pa, 'tkd%d_v' % b], w=['ps_oD%d_%d' % (b, hf)])
                            mm(ps_o[b][hf][:, ocol:ocol + 256], tTd[b][:, h, :], Sb[:, h, :], False, True,
                               r=['tTd%d' % b, 'Sb%d' % h], w=['ps_oD%d_%d' % (b, hf)])
                        pb = (i * 4 + h) % 2
                        mm(ps_s[pb][:, 0:256], tkd[b][:, kcol + h * 128:kcol + (h + 1) * 128], vh, True, True,
                           r=['tkd%d_k' % b, 'tkd%d_v' % b], w=['ps_sD%d' % pb])

                    def head_upd(h):
                        elh = eld[b][:, dr * 4 + h: dr * 4 + h + 1]
                        pb = (i * 4 + h) % 2
                        ts('dve', t1[h % 2][:], Sf[:, h, :], elh, None, ALU.mult, None, r=['Sf%d' % h, 'eld%d' % b], w=['t1D%d' % (h % 2)])
                        stt(Sf[:, h, :], ps_s[pb][:, 0:256], elh, t1[h % 2][:], ALU.mult, ALU.add,
                            r=['ps_sD%d' % pb, 'eld%d' % b, 't1D%d' % (h % 2)], w=['Sf%d' % h])
                        act(Sb[:, h, :], Sf[:, h, :], AF.Copy, r=['Sf%d' % h], w=['Sb%d' % h])

                    pas = [None] * 4
                    if full:
                        pas[0] = head_at(0)
                        pas[1] = head_at(1)
                    for h in range(4):
                        head_mid(h, pas[h])
                        if full and h + 2 < 4:
                            pas[h + 2] = head_at(h + 2)
                        if h >= 1:
                            head_upd(h - 1)
                    head_upd(3)
                    bg_issue(1, 'Sb3')
                    if not full:
                        return
                    if dr == 1:
                        for hf in range(2):
                            if hf == 0:
                                cp('dve', obs[b][:, 0:512], ps_o[b][0][:, :], r=['ps_oD%d_0' % b], w=['obs%d_0' % b])
                            else:
                                act(obs[b][:, 512:1024], ps_o[b][1][:, :], AF.Copy, r=['ps_oD%d_1' % b], w=['obs%d_1' % b])
                        st(ob_d[t - 16], obs[b][:], r=['obs%d_0' % b, 'obs%d_1' % b], w=['ob_d%d' % (t - 16)])
                        return
                    for hf in range(2):
                        tt('dve', osum[:, hf * 512:(hf + 1) * 512], ps_o[b][hf][:, :], obs[b][:, hf * 512:(hf + 1) * 512], ALU.add,
                           r=['ps_oD%d_%d' % (b, hf), 'obs%d' % b], w=['osum%d' % hf])
                    for h in range(4):
                        act(junk[:], osum[:, h * 256:(h + 1) * 256], AF.Square, r=['osum%d' % (h // 2)], w=['junkD', 'Dss%d' % h],
                            accum_out=st8[:, h:h + 1])
                    ts('dve', st8[:, 4:8], st8[:, 0:4], 1.0 / 256, EPS, ALU.mult, ALU.add, r=['Dss%d' % h for h in range(4)], w=['Dvar'])
                    act(st8[:, 8:12], st8[:, 4:8], AF.Sqrt, r=['Dvar'], w=['Dsd'])
                    S.op('dve', lambda e: e.reciprocal(out=st8[:, 12:16], in_=st8[:, 8:12]), r=['Dsd'], w=['Drstd'])
                    for h in range(4):
                        stt(og[:, h * 256:(h + 1) * 256], osum[:, h * 256:(h + 1) * 256], st8[:, 12 + h:13 + h],
                            gglab[:, h * 256:(h + 1) * 256], ALU.mult, ALU.mult,
                            r=['osum%d' % (h // 2), 'Drstd', 'gglab'], w=['og%d' % h])
                    tt('pool', catg[b][:], og[:], tkd[b][:, 2048:3072], ALU.mult, r=['og%d' % h for h in range(4)] + ['tkd%d_r' % b],
                       w=['catg%d' % b])
                    st(cat_d[t - 16][:, 0:1024], catg[b][:], r=['catg%d' % b], w=['cat_d%d_g' % (t - 16)])

                def reset_state():
                    S.op('dve', lambda e: e.memset(Sf[:], 0.0), r=[], w=['Sf%d' % h for h in range(4)])
                    S.op('pool', lambda e: e.memset(Sb[:], 0.0), r=[], w=['Sb%d' % h for h in range(4)])

                seq = [(t, 1, True) for t in range(31, 15, -1)] + [(t, 0, t >= 16) for t in range(32)]
                reset_state()
                gla_load(*seq[0])
                for n, item in enumerate(seq):
                    if n == 16:
                        reset_state()
                    if n + 1 < len(seq):
                        gla_load(*seq[n + 1])
                    gla_tile(*item)
                S.end_phase()

        bg_issue(1000)
        if 'E' in phases:
            with ExitStack() as pes:
                cosT = S.sb("cosT", [128, 3072], F32, pes)
                sinS = S.sb("sinS", [128, 3072], F32, pes)
                with ExitStack() as pes2:
                    posi = S.sb("posi", [128, 3072], I32, pes2)
                    ang = S.sb("ang", [128, 3072], F32, pes2)
                    kq = S.sb("kq", [128, 3072], I32, pes2)
                    rr_ = S.sb("rr_", [128, 3072], F32, pes2)
                    tm = S.sb("tm", [128, 3072], F32, pes2)
                    invf = S.sb("invf", [128, 1], F32, pes2)
                    sgn = S.sb("sgn", [128, 1], F32, pes2)
                    ld(invf[:], cst["invf"], w=['invf'])
                    ld(sgn[:], cst["sgn"], w=['sgn'])
                    ld(posi[:], pos_in[0, 1024:4096].partition_broadcast(128), w=['posi'])
                    cp('dve', tm[:], posi[:], r=['posi'], w=['tm'])
                    ts('dve', ang[:], tm[:], invf[:, 0:1], None, ALU.mult, None, r=['tm', 'invf'], w=['ang'])
                    ts('dve', tm[:], ang[:], 1.0 / TWO_PI, None, ALU.mult, None, r=['ang'], w=['tm'])
                    cp('dve', kq[:], tm[:], r=['tm'], w=['kq'])
                    cp('dve', tm[:], kq[:], r=['kq'], w=['tm'])
                    stt(rr_[:], tm[:], -TWO_PI, ang[:], ALU.mult, ALU.add, r=['tm', 'ang'], w=['rr_'])

                    def wrap(buf, name):
                        ts('dve', tm[:], buf[:], PI, -TWO_PI, ALU.is_gt, ALU.mult, r=[name], w=['tm'])
                        tt('dve', buf[:], buf[:], tm[:], ALU.add, r=[name, 'tm'], w=[name])
                        ts('dve', tm[:], buf[:], -PI, TWO_PI, ALU.is_lt, ALU.mult, r=[name], w=['tm'])
                        tt('dve', buf[:], buf[:], tm[:], ALU.add, r=[name, 'tm'], w=[name])
                        ts('dve', buf[:], buf[:], -PI, PI, ALU.max, ALU.min, r=[name], w=[name])

                    wrap(rr_, 'rr_')
                    act(sinS[:], rr_[:], AF.Sin, r=['rr_'], w=['sinS'])
                    ts('dve', sinS[:], sinS[:], sgn[:, 0:1], None, ALU.mult, None, r=['sinS', 'sgn'], w=['sinS'])
                    ts('dve', ang[:], rr_[:], PI / 2, None, ALU.add, None, r=['rr_'], w=['ang'])
                    wrap(ang, 'ang')
                    act(cosT[:], ang[:], AF.Sin, r=['ang'], w=['cosT'])
                    S.end_phase()
                pswap = S.sb("pswap", [128, 128], BF16, pes)
                pswf = S.sb("pswf", [128, 128], F32, pes)
                hblk = [S.sb("hblk%d" % i, [128, 4, 16, 128], BF16, pes) for i in range(2)]
                qb = [S.sb("qbE%d" % i, [128, 512], BF16, pes) for i in range(2)]
                r1 = [S.sb("r1E%d" % i, [128, 512], F32, pes) for i in range(2)]
                r2 = [S.sb("r2E%d" % i, [128, 512], F32, pes) for i in range(2)]
                rot = [S.sb("rotE%d" % i, [128, 512], BF16, pes) for i in range(2)]
                vst = [S.sb("vstE%d" % i, [128, 8, 129], BF16, pes) for i in range(2)]
                zt = S.sb("ztE", [128, 1032], BF16, pes)
                ps_p = [S.ps("ps_pE%d" % i, [128, 512], F32, pes) for i in range(2)]
                ps_w = [S.ps("ps_wE%d" % i, [128, 512], F32, pes) for i in range(2)]
                ps_v = [S.ps("ps_vE%d" % i, [128, 512], F32, pes) for i in range(2)]
                ld(pswf[:], cst["pswap"], w=['pswf'])
                cp('dve', pswap[:], pswf[:], r=['pswf'], w=['pswap'])
                S.op('pool', lambda e: e.memset(zt[:], 0.0), r=[], w=['ztE'])
                for i in range(2):
                    S.op('pool', lambda e, i=i: e.memset(vst[i][:], 1.0), r=[], w=['vstE%d' % i])
                for h in range(8):
                    st(kT_d[h][:, 3072:4096], zt[:, 0:1024], r=['ztE'], w=['kT_d_pad%d' % h])
                    st(v_d[3072 + h * 128:3072 + (h + 1) * 128, :], zt[:], r=['ztE'], w=['v_d_pad%d' % h])
                wd_all = ['wd%d_%d' % (kc, pc) for kc in range(16) for pc in range(3)]
                ucnt = [0]
                pendE = [None]
                for bk in range(6):
                    own = bk >= 2
                    b = bk % 2
                    for tl in range(4):
                        t = 8 + bk * 4 + tl
                        ld(hblk[b][:, tl], hT_d[t], r=['hT_d%d' % t], w=['hblk%d_%d' % (b, tl)])
                    hb_all = ['hblk%d_%d' % (b, tl) for tl in range(4)]
                    for h in range(8):
                        for which in ([1, 0] if own else [1]):
                            u = ucnt[0]
                            ucnt[0] += 1
                            pb = u % 2
                            cols = which * 1024 + h * 128
                            for kc in range(16):
                                mm(ps_p[pb][:, :], wd[:, kc, cols:cols + 128], hblk[b][:, :, kc, :], kc == 0, kc == 15,
                                   r=hb_all + wd_all, w=['ps_pE%d' % pb])
                            act(qb[pb][:], ps_p[pb][:, :], AF.Copy, r=['ps_pE%d' % pb], w=['qbE%d' % pb])
                            if pendE[0] is not None:
                                pendE[0]()

                            def rest(pb=pb, bk=bk, h=h, which=which):
                                mm(ps_w[pb][:, :], pswap[:], qb[pb][:], True, True, r=['pswap', 'qbE%d' % pb], w=['ps_wE%d' % pb])
                                tsl = slice(bk * 512, (bk + 1) * 512)
                                tt('dve', r1[pb][:], ps_p[pb][:, :], cosT[:, tsl], ALU.mult, r=['ps_pE%d' % pb, 'cosT'], w=['r1E%d' % pb])
                                tt('dve', r2[pb][:], ps_w[pb][:, :], sinS[:, tsl], ALU.mult, r=['ps_wE%d' % pb, 'sinS'], w=['r2E%d' % pb])
                                tt('pool', rot[pb][:], r1[pb][:], r2[pb][:], ALU.add, r=['r1E%d' % pb, 'r2E%d' % pb], w=['rotE%d' % pb])
                                if which == 1:
                                    st(kT_d[h][:, bk * 512:(bk + 1) * 512], rot[pb][:], r=['rotE%d' % pb], w=['kT_d%d_%d' % (h, bk)])
                                else:
                                    st(qT_d[h][:, (bk - 2) * 512:(bk - 1) * 512], rot[pb][:], r=['rotE%d' % pb], w=['qT_d%d_%d' % (h, bk)])
                            pendE[0] = rest
                    if pendE[0] is not None:
                        pendE[0]()
                        pendE[0] = None
                    for tl in range(4):
                        vb = (bk * 4 + tl) % 2
                        for c in range(2):
                            for kc in range(16):
                                mm(ps_v[c][:, :], hblk[b][:, tl, kc, :], wd[:, kc, 2048 + c * 512:2048 + (c + 1) * 512], kc == 0, kc == 15,
                                   r=hb_all + wd_all, w=['ps_vE%d' % c])
                            act(vst[vb][:, c * 4:(c + 1) * 4, 0:128], ps_v[c][:, :].rearrange("p (a b) -> p a b", a=4), AF.Copy,
                                r=['ps_vE%d' % c], w=['vstE%d_%d' % (vb, c)])
                        row0 = bk * 512 + tl * 128
                        st(v_d[row0:row0 + 128, :], vst[vb][:].rearrange("p a b -> p (a b)"),
                           r=['vstE%d' % vb, 'vstE%d_0' % vb, 'vstE%d_1' % vb], w=['v_d%d' % (bk * 4 + tl)])
                S.end_phase()

        pre_wd.close()
        pre_wo = ExitStack()
        wo = S.sb("wo", [128, 16, 2048], BF16, pre_wo)
        if 'G' in phases:
            for kc in range(16):
                for pc in range(2):
                    ld(wo[:, kc, pc * 1024:(pc + 1) * 1024], w_out[kc * 128:(kc + 1) * 128, pc * 1024:(pc + 1) * 1024],
                       w=['wo%d_%d' % (kc, pc)], q='pool')
        if 'F' in phases:
            with ExitStack() as pes:
                qTa = S.sb("qTa", [128, 8, 2048], BF16, pes)
                kTa = S.sb("kTa", [128, 8, 4096], BF16, pes)
                mf = S.sb("mfF", [128, 512], F32, pes)
                mAB = S.sb("mAB", [128, 256], BF16, pes)
                mABl = S.sb("mABl", [128, 256], BF16, pes)
                vA = [S.sb("vA%d" % i, [128, 1032], BF16, pes) for i in range(2)]
                vB = [S.sb("vB%d" % i, [128, 1032], BF16, pes) for i in range(2)]
                pexp = [S.sb("pexp%d" % i, [128, 256], BF16, pes) for i in range(4)]
                pm = [S.sb("pmF%d" % i, [128, 256], BF16, pes) for i in range(4)]
                ost = [S.sb("ostF%d" % i, [128, 1032], F32, pes) for i in range(2)]
                ps_s = [S.ps("ps_sF%d" % i, [128, 512], F32, pes) for i in range(4)]
                ps_o = [S.ps("ps_oF%d" % i, [128, 512], F32, pes) for i in range(4)]
                ld(mf[:, 0:256], cst["maskAB"], w=['mf0'])
                ld(mf[:, 256:512], cst["maskABl"], w=['mf1'])
                cp('dve', mAB[:], mf[:, 0:256], r=['mf0'], w=['mAB'])
                cp('dve', mABl[:], mf[:, 256:512], r=['mf1'], w=['mABl'])
                for h in range(8):
                    ld(qTa[:, h, :], qT_d[h], w=['qTa%d' % h])
                    ld(kTa[:, h, :], kT_d[h], w=['kTa%d' % h])
                bcnt = [0]
                ucnt = [0]
                pending = [None]

                def s_stage(b, h, kA, kB, DD, u0):
                    u = ucnt[0]
                    ucnt[0] += 1
                    pb = u % 4
                    qap = qTa[:, h, u0:u0 + 127 * DD + 1:DD]
                    mm(ps_s[pb][:, 0:128], kTa[:, h, kA:kA + 127 * DD + 1:DD], qap, True, True,
                       r=['qTa%d' % h, 'kTa%d' % h], w=['ps_sF%d' % pb])
                    mm(ps_s[pb][:, 128:256], kTa[:, h, kB:kB + 127 * DD + 1:DD], qap, True, True,
                       r=['qTa%d' % h, 'kTa%d' % h], w=['ps_sF%d' % pb])
                    return pb

                def o_stage(b, h, pb, msk, mname):
                    act(pexp[pb][:], ps_s[pb][:, 0:256], AF.Exp, r=['ps_sF%d' % pb], w=['pexp%d' % pb], scale=128.0 ** -0.5)
                    tt('dve', pm[pb][:], pexp[pb][:], msk[:], ALU.mult, r=['pexp%d' % pb, mname], w=['pmF%d' % pb])
                    mm(ps_o[pb][:, 0:129], pm[pb][:, 0:128], vA[b][:, h * 129:(h + 1) * 129], True, False,
                       r=['pmF%d' % pb, 'vA%d' % b], w=['ps_oF%d' % pb])
                    mm(ps_o[pb][:, 0:129], pm[pb][:, 128:256], vB[b][:, h * 129:(h + 1) * 129], False, True,
                       r=['pmF%d' % pb, 'vB%d' % b], w=['ps_oF%d' % pb])

                def c_stage(b, h, pb):
                    act(ost[b][:, h * 129:(h + 1) * 129], ps_o[pb][:, 0:129], AF.Copy, r=['ps_oF%d' % pb], w=['ostF%d_%d' % (b, h)])

                blocks = []
                for pi, DD in enumerate([1, 4, 16]):
                    nsp = 16 // DD
                    for sp_i in range(nsp):
                        for rs in range(DD):
                            u0 = 128 * DD * sp_i + rs
                            kA = 1024 + u0 - 64 * DD
                            blocks.append((pi, DD, u0, kA, kA + 128 * DD, sp_i == nsp - 1))

                def f_load(bi):
                    pi, DD, u0, kA, kB, last = blocks[bi]
                    b = bi % 2
                    ld(vA[b][:], v_d[kA:kA + 127 * DD + 1:DD, :], w=['vA%d' % b])
                    ld(vB[b][:], v_d[kB:kB + 127 * DD + 1:DD, :], w=['vB%d' % b])

                pend = []
                pendc = []
                f_load(0)
                for bi, (pi, DD, u0, kA, kB, last) in enumerate(blocks):
                    b = bi % 2
                    msk, mname = (mABl, 'mABl') if last else (mAB, 'mAB')
                    for h in range(8):
                        pb = s_stage(b, h, kA, kB, DD, u0)
                        pend.append(lambda b=b, h=h, pb=pb, msk=msk, mname=mname: o_stage(b, h, pb, msk, mname))
                        pendc.append(lambda b=b, h=h, pb=pb, pi=pi, u0=u0, DD=DD, bi=bi:
                                     (c_stage(b, h, pb),
                                      st(oacc_d[pi][u0:u0 + 127 * DD + 1:DD, :], ost[b][:], r=['ostF%d_%d' % (b, hh) for hh in range(8)],
                                         w=['oacc_d%d' % bi]) if h == 7 else None))
                        if len(pend) > 2:
                            pend.pop(0)()
                        if len(pendc) > 4:
                            pendc.pop(0)()
                        if h == 4 and bi + 1 < len(blocks):
                            f_load(bi + 1)
                while pend:
                    pend.pop(0)()
                while pendc:
                    pendc.pop(0)()
                S.end_phase()

        if 'G' in phases:
            with ExitStack() as pes:
                gaab = S.sb("gaab", [128, 2048], F32, pes)
                oa = [[S.sb("oaG%d_%d" % (i, p), [128, 1032], F32, pes) for p in range(3)] for i in range(2)]
                rden = [S.sb("rdenG%d" % i, [128, 8], F32, pes) for i in range(2)]
                cat = [S.sb("catG%d" % i, [128, 2048], BF16, pes) for i in range(2)]
                catT = [S.sb("catTG%d" % i, [128, 16, 128], BF16, pes) for i in range(2)]
                xg = [S.sb("xG%d" % i, [128, 2048], F32, pes) for i in range(2)]
                tmpg = S.sb("tmpG", [128, 2048], F32, pes)
                x1s = [S.sb("x1sG%d" % i, [128, 2048], F32, pes) for i in range(2)]
                ps_tr = [S.ps("ps_trG%d" % i, [128, 1024], BF16, pes) for i in range(2)]
                ps_m = [S.ps("ps_mG%d" % i, [128, 512], F32, pes) for i in range(4)]
                wo_all = ['wo%d_%d' % (kc, pc) for kc in range(16) for pc in range(2)]
                ld(gaab[:], mod_flat[4096:6144].partition_broadcast(128), w=['gaab'])
                def g_stage1a(t):
                    b = t % 2
                    for p in range(3):
                        ld(oa[b][p][:], oacc_d[p][t * 128:(t + 1) * 128, :], w=['oaG%d_%d' % (b, p)])
                    ld(cat[b][:, 0:1024], cat_d[t][:, 0:1024], w=['catG%d_g' % b])
                    ld(xg[b][:], x_loc[OWN0 + t * 128:OWN0 + (t + 1) * 128, :], w=['xG%d' % b])
                    tt('pool', oa[b][0][:], oa[b][0][:], oa[b][1][:], ALU.add, r=['oaG%d_0' % b, 'oaG%d_1' % b], w=['oaG%d_0' % b])
                    tt('pool', oa[b][0][:], oa[b][0][:], oa[b][2][:], ALU.add, r=['oaG%d_0' % b, 'oaG%d_2' % b], w=['oaG%d_0' % b])

                def g_stage1b(t):
                    b = t % 2
                    acc3 = oa[b][0][:].rearrange("p (a c) -> p a c", a=8)
                    S.op('dve', lambda e, acc3=acc3, b=b: e.reciprocal(out=rden[b][:], in_=acc3[:, :, 128]), r=['oaG%d_0' % b], w=['rdenG%d' % b])
                    for h in range(8):
                        ts('dve', cat[b][:, 1024 + h * 128:1024 + (h + 1) * 128], acc3[:, h, 0:128], rden[b][:, h:h + 1], None, ALU.mult, None,
                           r=['oaG%d_0' % b, 'rdenG%d' % b], w=['catG%d_d%d' % (b, h)])

                def g_stage2a(t):
                    b = t % 2
                    cat_all = ['catG%d_g' % b] + ['catG%d_d%d' % (b, h) for h in range(8)]
                    for kc in range(16):
                        hf, k8 = kc // 8, kc % 8
                        tr(ps_tr[hf][:, k8 * 128:(k8 + 1) * 128], cat[b][:, kc * 128:(kc + 1) * 128], ident_b[:],
                           r=cat_all + ['ident_b'], w=['ps_trG%d' % hf])
                    cp('dve', catT[b][:, 0:8, :], ps_tr[0][:, :].rearrange("p (a c) -> p a c", a=8), r=['ps_trG0'], w=['catTG%d_0' % b])
                    act(catT[b][:, 8:16, :], ps_tr[1][:, :].rearrange("p (a c) -> p a c", a=8), AF.Copy, r=['ps_trG1'], w=['catTG%d_1' % b])

                def g_stage2b(t):
                    b = t % 2
                    for n in range(4):
                        for kc in range(16):
                            mm(ps_m[n][:, :], catT[b][:, kc, :], wo[:, kc, n * 512:(n + 1) * 512], kc == 0, kc == 15,
                               r=['catTG%d_0' % b, 'catTG%d_1' % b] + wo_all, w=['ps_mG%d' % n])
                        nsl = slice(n * 512, (n + 1) * 512)
                        tt('dve', tmpg[:, nsl], ps_m[n][:, :], gaab[:, nsl], ALU.mult, r=['ps_mG%d' % n, 'gaab'], w=['tmpG%d' % n])
                        tt('dve', x1s[b][:, nsl], tmpg[:, nsl], xg[b][:, nsl], ALU.add, r=['tmpG%d' % n, 'xG%d' % b], w=['x1sG%d_%d' % (b, n)])
                    st(x1_d[t], x1s[b][:], r=['x1sG%d_%d' % (b, n) for n in range(4)], w=['x1_d%d' % t])

                g_stage1a(0)
                g_stage1b(0)
                for t in range(16):
                    if t + 1 < 16:
                        g_stage1a(t + 1)
                    g_stage2a(t)
                    if t + 1 < 16:
                        g_stage1b(t + 1)
                    g_stage2b(t)
                S.end_phase()

        pre_wo.close()
        if 'H' in phases:
            S.barrier(include_bg=True)
            with ExitStack() as pes:
                NBR = 5
                NBV = 5
                HT = int(os.environ.get("HTILES", "16"))
                wq = S.sb("wq", [128, 16, 2048], BF16, pes)
                keysb = S.sb("keysb", [128, 16, 128], BF16, pes)
                gafb = S.sb("gafb", [128, 2048], F32, pes)
                gfinb = S.sb("gfinb", [128, 2048], F32, pes)
                iota16 = S.sb("iota16", [128, 16], F32, pes)
                x1t = S.sb("x1t", [128, 2048], F32, pes)
                x1e = S.sb("x1e", [128, 2048], F32, pes)
                tmpE = S.sb("tmpE", [128, 512], F32, pes)
                xnb = S.sb("xnbH", [128, 2048], BF16, pes)
                h2T = S.sb("h2T", [128, 16, 128], BF16, pes)
                h2b = [S.sb("h2b%d" % i, [128, 2048], BF16, pes) for i in range(2)]
                qTs = S.sb("qTs", [128, 16, 128], BF16, pes)
                sc = S.sb("scH", [128, 16, 128], F32, pes)
                sc2 = [S.sb("sc2H%d" % i, [128, 128], F32, pes) for i in range(2)]
                tv = S.sb("tvH", [128, 16, 16], F32, pes)
                ti = S.sb("tiH", [128, 16, 16], U32, pes)
                tif = S.sb("tifH", [128, 16, 16], F32, pes)
                cand = [S.sb("candH%d" % i, [128, 16, 16], F32, pes) for i in range(2)]
                cand2 = [S.sb("cand2H%d" % i, [128, 256], F32, pes) for i in range(2)]
                bs = S.sb("bsH", [128, 8, 16], F32, pes)
                posu = S.sb("posuH", [128, 8, 16], U32, pes)
                au = S.sb("auH", [128, 128], U32, pes)
                bu = S.sb("buH", [128, 128], U32, pes)
                af = S.sb("afH", [128, 128], F32, pes)
                bf = S.sb("bfH", [128, 128], F32, pes)
                i0 = S.sb("i0H", [128, 128], F32, pes)
                i1 = S.sb("i1H", [128, 128], F32, pes)
                idxf = S.sb("idxfH", [128, 128], F32, pes)
                idxu = [S.sb("idxuH%d" % i, [128, 128], U32, pes) for i in range(3)]
                nm = S.sb("nmH", [128, 8], F32, pes)
                Zs = S.sb("ZsH", [128, 8], F32, pes)
                rZ = S.sb("rZH", [128, 8], F32, pes)
                ge = S.sb("geH", [128, 8, 16], F32, pes)
                gate = [S.sb("gateH%d" % i, [128, 8, 16], F32, pes) for i in range(3)]
                a_all = [S.sb("a_allH%d" % i, [128, 128], F32, pes) for i in range(2)]
                ga = S.sb("gaH", [128, 128], F32, pes)
                wgt = [S.sb("wgtH%d" % i, [128, 128], F32, pes) for i in range(2)]
                gbU = [S.sb("gbU%d" % i, [128, 2048], BF16, pes) for i in range(NBR)]
                gbV = [S.sb("gbV%d" % i, [128, 2048], BF16, pes) for i in range(NBV)]
                prod = [S.sb("prodH%d" % i, [128, 2048], BF16, pes) for i in range(2)]
                pcn = [0]
                dg = [S.sb("dgH%d" % i, [128, 128], BF16, pes) for i in range(4)]
                junkb = S.sb("junkbH", [128, 2048], BF16, pes)
                junka = S.sb("junkaH", [128, 2048], BF16, pes)
                st4 = S.sb("st4H", [128, 8], F32, pes)
                ps_pro = [S.ps("ps_proH%d" % i, [128, 512], F32, pes) for i in range(4)]
                ps_big = [S.ps("ps_bigH%d" % i, [128, 512], F32, pes) for i in range(4)]
                for kc in range(16):
                    for pc in range(2):
                        ld(wq[:, kc, pc * 1024:(pc + 1) * 1024], w_pq[kc * 128:(kc + 1) * 128, pc * 1024:(pc + 1) * 1024],
                           w=['wq%d_%d' % (kc, pc)], q='pool')
                wq_all = ['wq%d_%d' % (kc, pc) for kc in range(16) for pc in range(2)]
                for pc in range(2):
                    ld(keysb[:, pc * 8:(pc + 1) * 8, :], keysT_in[:, pc * 8:(pc + 1) * 8, :], w=['keysb%d' % pc], q='pool')
                ld(iota16[:], cst["iota16"], w=['iota16'])
                ld(gafb[:], mod_flat[10240:12288].partition_broadcast(128), w=['gafb'])
                ld(gfinb[:], gfin_row[0, :].partition_broadcast(128), w=['gfinb'])
                pa = list(tv[:].ap[0])

                def bcast_ap(tile_ap, dims):
                    return bass.AP(tile_ap.tensor, tile_ap.offset, [list(tile_ap.ap[0])] + dims)

                def pro_bf(i):
                    return ps_pro[i][:, :].bitcast(BF16)

                def prologue(t):
                    s2, s3 = t % 2, t % 3
                    ld(x1t[:], x1_d[t], r=['x1_d%d' % t], w=['x1t'])
                    act(junka[:], x1t[:], AF.Square, r=['x1t'], w=['junkaH', 'H0ss'], accum_out=st4[:, 0:1])
                    rstd_from_ss(st4[:, 0:1], st4[:, 1:2], st4[:, 2:3], st4[:, 3:4], D, 'H0')
                    act(xnb[:], x1t[:], AF.Copy, r=['x1t', 'H0rstd'], w=['xnbH'], scale=st4[:, 3:4])
                    yield
                    for kc in range(16):
                        hf, k8 = kc // 8, kc % 8
                        tr(pro_bf(hf)[:, k8 * 128:(k8 + 1) * 128], xnb[:, kc * 128:(kc + 1) * 128], ident_b[:],
                           r=['xnbH', 'ident_b'], w=['ps_proH%d' % hf])
                        if kc % 4 == 3:
                            yield
                    for kc in range(16):
                        hf, k8 = kc // 8, kc % 8
                        if hf == 0:
                            ts('dve', h2T[:, kc, :], pro_bf(hf)[:, k8 * 128:(k8 + 1) * 128], gscfT[:, kc:kc + 1], modT[:, 48 + kc:49 + kc],
                               ALU.mult, ALU.add, r=['ps_proH0', 'gscfT', 'modT'], w=['h2T_%d' % kc])
                        else:
                            act(h2T[:, kc, :], pro_bf(hf)[:, k8 * 128:(k8 + 1) * 128], AF.Identity, r=['ps_proH1', 'gscfT', 'modT'],
                                w=['h2T_%d' % kc], bias=modT[:, 48 + kc:49 + kc], scale=gscfT[:, kc:kc + 1])
                        if kc % 4 == 3:
                            yield
                    h2T_all = ['h2T_%d' % kc for kc in range(16)]
                    for kc in range(16):
                        hf, k8 = kc // 8, kc % 8
                        tr(pro_bf(2 + hf)[:, k8 * 128:(k8 + 1) * 128], h2T[:, kc, :], ident_b[:],
                           r=['h2T_%d' % kc, 'ident_b'], w=['ps_proH%d' % (2 + hf)])
                        if kc % 4 == 3:
                            yield
                    cp('dve', h2b[s2][:, 0:1024], pro_bf(2)[:, :], r=['ps_proH2'], w=['h2b%d_0' % s2])
                    act(h2b[s2][:, 1024:2048], pro_bf(3)[:, :], AF.Copy, r=['ps_proH3'], w=['h2b%d_1' % s2])
                    yield
                    for c in range(16):
                        pb = c % 2
                        for kc in range(16):
                            mm(ps_pro[pb][:, 0:128], wq[:, kc, c * 128:(c + 1) * 128], h2T[:, kc, :], kc == 0, kc == 15,
                               r=h2T_all + wq_all, w=['ps_proH%d' % pb])
                        yield
                        if pb == 0:
                            act(qTs[:, c, :], ps_pro[pb][:, 0:128], AF.Copy, r=['ps_proH0'], w=['qTs%d' % c])
                        else:
                            cp('dve', qTs[:, c, :], ps_pro[pb][:, 0:128], r=['ps_proH1'], w=['qTs%d' % c])
                        mm(ps_pro[2 + pb][:, 0:128], qTs[:, c, :], keysb[:, c, :], True, True,
                           r=['qTs%d' % c, 'keysb0', 'keysb1'], w=['ps_proH%d' % (2 + pb)])
                        if pb == 0:
                            cp('dve', sc[:, c, :], ps_pro[2][:, 0:128], r=['ps_proH2'], w=['scH%d' % c])
                        else:
                            act(sc[:, c, :], ps_pro[3][:, 0:128], AF.Copy, r=['ps_proH3'], w=['scH%d' % c])
                        yield
                    for c in range(16):
                        rb = c % 2
                        sn = 'scH%d' % c
                        S.op('dve', lambda e, c=c: e.max(out=tv[:, c, 0:8], in_=sc[:, c, :]), r=[sn], w=['tvH%d_a' % c])
                        S.op('dve', lambda e, c=c: e.max_index(out=ti[:, c, 0:8], in_max=tv[:, c, 0:8], in_values=sc[:, c, :]),
                             r=[sn, 'tvH%d_a' % c], w=['tiH%d_a' % c])
                        S.op('dve', lambda e, c=c, rb=rb: e.match_replace(out=sc2[rb][:], in_to_replace=tv[:, c, 0:8], in_values=sc[:, c, :],
                                                                       imm_value=-1e30), r=[sn, 'tvH%d_a' % c], w=['sc2H%d' % rb])
                        S.op('dve', lambda e, c=c, rb=rb: e.max(out=tv[:, c, 8:16], in_=sc2[rb][:]), r=['sc2H%d' % rb], w=['tvH%d_b' % c])
                        S.op('dve', lambda e, c=c, rb=rb: e.max_index(out=ti[:, c, 8:16], in_max=tv[:, c, 8:16], in_values=sc2[rb][:]),
                             r=['sc2H%d' % rb, 'tvH%d_b' % c], w=['tiH%d_b' % c])
                        yield
                    tv_all = ['tvH%d_%s' % (c, x) for c in range(16) for x in 'ab']
                    ti_all = ['tiH%d_%s' % (c, x) for c in range(16) for x in 'ab']
                    cp('dve', tif[:], ti[:], r=ti_all, w=['tifH'])
                    for h in range(8):
                        rb = h % 2
                        in0 = bcast_ap(tv[:, 2 * h, :], [[1, 16], [0, 16]])
                        in1 = bcast_ap(tv[:, 2 * h + 1, :], [[0, 16], [1, 16]])
                        tt('dve', cand[rb][:], in0, in1, ALU.add, r=tv_all, w=['candH%d' % rb])
                        cf = cand[rb][:].rearrange("p a b -> p (a b)")
                        S.op('dve', lambda e, h=h, cf=cf: e.max(out=bs[:, h, 0:8], in_=cf), r=['candH%d' % rb], w=['bsH%d_a' % h])
                        S.op('dve', lambda e, h=h, cf=cf: e.max_index(out=posu[:, h, 0:8], in_max=bs[:, h, 0:8], in_values=cf),
                             r=['candH%d' % rb, 'bsH%d_a' % h], w=['posuH%d_a' % h])
                        S.op('dve', lambda e, h=h, cf=cf, rb=rb: e.match_replace(out=cand2[rb][:], in_to_replace=bs[:, h, 0:8], in_values=cf,
                                                                              imm_value=-1e30), r=['candH%d' % rb, 'bsH%d_a' % h], w=['cand2H%d' % rb])
                        S.op('dve', lambda e, h=h, rb=rb: e.max(out=bs[:, h, 8:16], in_=cand2[rb][:]), r=['cand2H%d' % rb], w=['bsH%d_b' % h])
                        S.op('dve', lambda e, h=h, rb=rb: e.max_index(out=posu[:, h, 8:16], in_max=bs[:, h, 8:16], in_values=cand2[rb][:]),
                             r=['cand2H%d' % rb, 'bsH%d_b' % h], w=['posuH%d_b' % h])
                        yield
                    bs_all = ['bsH%d_%s' % (h, x) for h in range(8) for x in 'ab']
                    pos_all = ['posuH%d_%s' % (h, x) for h in range(8) for x in 'ab']
                    posf = posu[:].rearrange("p a b -> p (a b)")
                    S.op('dve', lambda e: e.tensor_single_scalar(out=au[:], in_=posf, scalar=4, op=ALU.logical_shift_right), r=pos_all, w=['auH'])
                    S.op('dve', lambda e: e.tensor_single_scalar(out=bu[:], in_=posf, scalar=15, op=ALU.bitwise_and), r=pos_all, w=['buH'])
                    cp('dve', af[:], au[:], r=['auH'], w=['afH'])
                    cp('dve', bf[:], bu[:], r=['buH'], w=['bfH'])
                    yield
                    eq3 = x1t[:].rearrange("p (a b) -> p a b", b=16)
                    eq4 = x1t[:].rearrange("p (h k b) -> p h k b", h=8, k=16)
                    io_b = bcast_ap(iota16[:], [[0, 128], [1, 16]])
                    for sidx, (xf, xname, iout, iname) in enumerate([(af, 'afH', i0, 'i0H'), (bf, 'bfH', i1, 'i1H')]):
                        xb = bcast_ap(xf[:], [[1, 128], [0, 16]])
                        tt('dve', eq3, xb, io_b, ALU.is_equal, r=[xname, 'iota16'], w=['x1t'])
                        yield
                        tib = bass.AP(tif[:].tensor, tif[:].offset + sidx * 16, [pa, [32, 8], [0, 16], [1, 16]])
                        tt('dve', eq4, eq4, tib, ALU.mult, r=['x1t', 'tifH'], w=['x1t'])
                        yield
                        S.op('dve', lambda e, iout=iout: e.tensor_reduce(out=iout[:], in_=eq3, axis=AX.X, op=ALU.add), r=['x1t'], w=[iname])
                        yield
                    stt(idxf[:], i0[:], 128.0, i1[:], ALU.mult, ALU.add, r=['i0H', 'i1H'], w=['idxfH'])
                    cp('dve', idxu[s3][:], idxf[:], r=['idxfH'], w=['idxuH%d' % s3])
                    ts('dve', nm[:], bs[:, :, 0], -1.0, None, ALU.mult, None, r=bs_all, w=['nmH'])
                    for h in range(8):
                        act(ge[:, h, :], bs[:, h, :], AF.Exp, r=bs_all + ['nmH'], w=['geH%d' % h, 'ZsH%d' % h], bias=nm[:, h:h + 1], scale=1.0,
                            accum_out=Zs[:, h:h + 1])
                    S.op('dve', lambda e: e.reciprocal(out=rZ[:], in_=Zs[:]), r=['ZsH%d' % h for h in range(8)], w=['rZH'])
                    tt('dve', gate[s3][:], ge[:], bcast_ap(rZ[:], [[1, 8], [0, 16]]), ALU.mult, r=['geH%d' % h for h in range(8)] + ['rZH'],
                       w=['gateH%d' % s3])
                    yield

                gcU = [0]
                gcV = [0]
                dcnt = [0]

                def down_step(t, k):
                    s2, s3 = t % 2, t % 3
                    rg = gcU[0] % NBR
                    gcU[0] += 1
                    S.dma('pool', lambda e, rg=rg, k=k, s3=s3: e.indirect_dma_start(
                        out=gbU[rg][:], out_offset=None, in_=ub_d,
                        in_offset=bass.IndirectOffsetOnAxis(ap=idxu[s3][:, k:k + 1], axis=0)), r=['idxuH%d' % s3], w=['gbU%d' % rg])
                    h2n = ['h2b%d_0' % s2, 'h2b%d_1' % s2]
                    if k % 3 != 0:
                        pp = pcn[0] % 2
                        pcn[0] += 1
                        tt('dve', prod[pp][:], gbU[rg][:], h2b[s2][:], ALU.mult, r=['gbU%d' % rg] + h2n, w=['prodH%d' % pp])
                        act(junka[:, 0:2048], prod[pp][:], AF.Copy, r=['prodH%d' % pp], w=['junkaH', 'a_allH%d_%d' % (s2, k)],
                            accum_out=a_all[s2][:, k:k + 1])
                    else:
                        S.op('dve', lambda e, rg=rg, k=k, s2=s2: e.scalar_tensor_tensor(
                            out=junkb[:], in0=gbU[rg][:], scalar=1.0, in1=h2b[s2][:], op0=ALU.mult, op1=ALU.mult,
                            accum_out=a_all[s2][:, k:k + 1]), r=['gbU%d' % rg] + h2n, w=['junkbH', 'a_allH%d_%d' % (s2, k)])

                def up_step(t, k):
                    s2, s3 = t % 2, t % 3
                    rg = gcV[0] % NBV
                    gcV[0] += 1
                    dd = dcnt[0] % 4
                    dcnt[0] += 1
                    S.dma('pool', lambda e, rg=rg, k=k, s3=s3: e.indirect_dma_start(
                        out=gbV[rg][:], out_offset=None, in_=vb_d,
                        in_offset=bass.IndirectOffsetOnAxis(ap=idxu[s3][:, k:k + 1], axis=0)), r=['idxuH%d' % s3], w=['gbV%d' % rg])
                    act(dg[dd][:], ident_f[:], AF.Copy, r=['ident_f', 'wgtH%d' % s2], w=['dgH%d' % dd], scale=wgt[s2][:, k:k + 1])
                    for n in range(4):
                        mm(ps_big[n][:, :], dg[dd][:], gbV[rg][:, n * 512:(n + 1) * 512], k == 0, k == 127,
                           r=['dgH%d' % dd, 'gbV%d' % rg], w=['ps_bigH%d' % n])

                def finish_down(t):
                    s2, s3 = t % 2, t % 3
                    a_names = ['a_allH%d_%d' % (s2, k) for k in range(128)]
                    act(ga[:], a_all[s2][:], AF.Gelu, r=a_names, w=['gaH'])
                    tt('dve', wgt[s2][:], ga[:], gate[s3][:].rearrange("p a b -> p (a b)"), ALU.mult, r=['gaH', 'gateH%d' % s3], w=['wgtH%d' % s2])

                def epilogue(t):
                    for n in range(4):
                        nsl = slice(n * 512, (n + 1) * 512)
                        tt('dve', tmpE[:], ps_big[n][:, :], gafb[:, nsl], ALU.mult, r=['ps_bigH%d' % n, 'gafb'], w=['tmpE'])
                        tt('dve', x1e[:, nsl], x1e[:, nsl], tmpE[:], ALU.add, r=['x1e', 'tmpE'], w=['x1e'])
                    act(junka[:], x1e[:], AF.Square, r=['x1e'], w=['junkaH', 'H1ss'], accum_out=st4[:, 4:5])
                    rstd_from_ss(st4[:, 4:5], st4[:, 5:6], st4[:, 6:7], st4[:, 7:8], D, 'H1')
                    stt(x1e[:], x1e[:], st4[:, 7:8], gfinb[:], ALU.mult, ALU.mult, r=['x1e', 'H1rstd', 'gfinb'], w=['x1e'])
                    st(out_d[t * 128:(t + 1) * 128, :], x1e[:], r=['x1e'], w=['out_d%d' % t], final=True)

                for _ in prologue(0):
                    pass
                for i in range(HT + 1):
                    gen = prologue(i + 1) if i + 1 < HT else None
                    if i >= 1:
                        ld(x1e[:], x1_d[i - 1], r=['x1_d%d' % (i - 1)], w=['x1e'])
                    for k in range(128):
                        if i < HT:
                            down_step(i, k)
                        if i >= 1:
                            up_step(i - 1, k)
                        if gen is not None:
                            try:
                                next(gen)
                            except StopIteration:
                                gen = None
                    if gen is not None:
                        for _ in gen:
                            pass
                    if i < HT:
                        finish_down(i)
                    if i >= 1:
                        epilogue(i - 1)
                S.end_phase()

        S.finish()
    return nc


def prep_inputs(inputs):
    g = {k: np.asarray(v) for k, v in inputs.items()}
    l = 0
    consts = _consts()
    w_in = np.ascontiguousarray(g["w_in"][l])
    keysT = np.ascontiguousarray(g["peer_sub_keys"][l].reshape(16, 128, 128).transpose(2, 0, 1))
    shared = {
        "w_ada": np.ascontiguousarray(g["w_ada"][l]),
        "b_ada_row": np.ascontiguousarray(g["b_ada"][l].reshape(1, 6 * D)),
        "g_mixT": np.ascontiguousarray(g["g_norm_mix"][l].reshape(16, 128).T),
        "g_ffnT": np.ascontiguousarray(g["g_norm_ffn"][l].reshape(16, 128).T),
        "g_ffn_row": np.ascontiguousarray(g["g_norm_ffn"][l].reshape(1, D)),
        "g_fin_row": np.ascontiguousarray(g["g_final"].reshape(1, D)),
        "g_gla_row": np.ascontiguousarray(g["g_gla_out"][l].reshape(1, 1024)),
        "w_in": w_in,
        "w_out": np.ascontiguousarray(g["w_out"][l]),
        "w_pq": np.ascontiguousarray(g["w_peer_q"][l]),
        "keysT": keysT,
        "peer_u": np.ascontiguousarray(g["peer_u"][l]),
        "peer_v": np.ascontiguousarray(g["peer_v"][l]),
    }
    for k, v in consts.items():
        shared["c_" + k] = v
    zeros = np.zeros((16, 512), np.float32)
    wgf, wgb = g["w_gate_f"][l], g["w_gate_b"][l]
    bgf, bgb = g["b_gate_f"][l], g["b_gate_b"][l]
    gzf_cols = w_in[:, 3072:3088]
    gzb_cols = w_in[:, 3088:3104]
    in_maps = []
    for b in range(4):
        for j in range(2):
            m = dict(shared)
            xb = g["x"][b]
            pb = g["positions"][b].astype(np.int32)
            if j == 0:
                xb = xb[::-1]
                pb = pb[::-1]
                f_w, f_b, f_cols = wgb, bgb, gzb_cols
                b_w, b_b, b_cols = wgf, bgf, gzf_cols
            else:
                f_w, f_b, f_cols = wgf, bgf, gzf_cols
                b_w, b_b, b_cols = wgb, bgb, gzb_cols
            m["x_loc"] = np.ascontiguousarray(xb)
            m["pos"] = np.ascontiguousarray(pb.reshape(1, NTOK))
            m["cT"] = np.ascontiguousarray(g["c"][b].reshape(16, 128).T)
            m["w_gz"] = np.ascontiguousarray(np.concatenate([f_cols, b_cols], axis=1))
            m["Wg"] = np.ascontiguousarray(np.concatenate([
                np.concatenate([f_w, zeros], axis=1),
                np.concatenate([zeros, b_w], axis=1),
                np.concatenate([f_b, b_b])[None, :]], axis=0).astype(np.float32))
            in_maps.append(m)
    return in_maps


def kernel(**inputs):
    in_maps = prep_inputs(inputs)
    nc = build()
    res = run_bass_kernel_spmd(nc, in_maps, core_ids=list(range(8)))
    out = np.empty((4, 4096, D), np.float32)
    for b in range(4):
        for j in range(2):
            o = np.asarray(res.results[b * 2 + j]["out"])
            if j == 0:
                out[b, :2048] = o[::-1]
            else:
                out[b, 2048:] = o
    return out
```
